# Optimizing a Trainium2 kernel written in Bass

```python
import jax, jax.numpy as jnp
from jax import lax
import numpy as np

D_MODEL = 4096
BATCH = 2
SEQ = 8192
DEPTH = 2

HEAD_DIM = 128
N_BRANCH = 4
MIX_WIDTH = D_MODEL // N_BRANCH
N_HEADS = MIX_WIDTH // HEAD_DIM
CONV_WIDTH = 4
MLSTM_CHUNK = 128
RET_CHUNK = 128
NSA_KV_GROUPS = 2
NSA_GROUP_SIZE = N_HEADS // NSA_KV_GROUPS
KV_WIDTH = NSA_KV_GROUPS * HEAD_DIM
CMP_BLOCK = 32
CMP_STRIDE = 16
SEL_BLOCK = 64
SEL_TOPN = 16
WINDOW = 512
Q_BLOCK = 128
SGU_CHUNK = 128
N_GROUPS = 8
EXPERTS_PER_GROUP = 8
N_EXPERTS = N_GROUPS * EXPERTS_PER_GROUP
TOP_K_IN_GROUP = 2
EXPERT_FF = 256
MOE_BLOCK = 128
RMS_EPS = 1e-6
NEG_INF = -1e30
FORCE_SCORE = 1e9

COL_WIDTHS = (
    MIX_WIDTH, MIX_WIDTH, MIX_WIDTH, MIX_WIDTH, N_HEADS, N_HEADS,
    MIX_WIDTH, MIX_WIDTH, MIX_WIDTH, MIX_WIDTH,
    MIX_WIDTH, KV_WIDTH, KV_WIDTH, KV_WIDTH, KV_WIDTH, KV_WIDTH, KV_WIDTH, 3 * N_HEADS,
    MIX_WIDTH, MIX_WIDTH,
    N_BRANCH * D_MODEL,
)
C_IN = sum(COL_WIDTHS)

kernel_name = "hybrid_mlstm_retnet_nsa_sgu_hiermoe"


def rmsnorm(x, g):
    xf = x.astype(jnp.float32)
    y = xf * lax.rsqrt(jnp.mean(xf * xf, axis=-1, keepdims=True) + RMS_EPS)
    return (y * g.astype(jnp.float32)).astype(x.dtype)


def head_norm(h, g, dtype):
    B, T, H, d = h.shape
    hf = h.astype(jnp.float32)
    hf = hf * lax.rsqrt(jnp.mean(hf * hf, axis=-1, keepdims=True) + RMS_EPS)
    return (hf.reshape(B, T, H * d) * g.astype(jnp.float32)).astype(dtype)


def alibi_slopes(n):
    return 2.0 ** (-8.0 * (jnp.arange(n, dtype=jnp.float32) + 1.0) / n)


def split_columns(z):
    idx, off = [], 0
    for w in COL_WIDTHS[:-1]:
        off += w
        idx.append(off)
    return jnp.split(z, idx, axis=-1)


def causal_conv(x, w):
    K, C = w.shape
    return lax.conv_general_dilated(x, w[:, None, :].astype(x.dtype), window_strides=(1,),
                                    padding=[(K - 1, 0)], dimension_numbers=('NWC', 'WIO', 'NWC'),
                                    feature_group_count=C)


def mlstm(q, k, v, i_pre, f_pre):
    B, T, _ = q.shape
    H, d, L = N_HEADS, HEAD_DIM, MLSTM_CHUNK
    nc = T // L
    f32 = jnp.float32

    def heads(a):
        return a.astype(f32).reshape(B, nc, L, H, d).transpose(1, 0, 3, 2, 4)

    def gate(a):
        return a.astype(f32).reshape(B, nc, L, H).transpose(1, 0, 3, 2)

    qc, kc, vc = heads(q) * (d ** -0.5), heads(k), heads(v)
    ic, fc = gate(i_pre), jax.nn.log_sigmoid(gate(f_pre))
    causal = jnp.tril(jnp.ones((L, L), dtype=bool))

    def step(carry, inp):
        c_mem, n_mem, m_mem = carry
        qb, kb, vb, ib, fb = inp
        bcum = jnp.cumsum(fb, axis=-1)
        log_d = bcum[..., :, None] - bcum[..., None, :] + ib[..., None, :]
        log_d = jnp.where(causal, log_d, NEG_INF)
        log_inter = bcum + m_mem[..., None]
        m_row = jnp.maximum(jnp.max(log_d, axis=-1), log_inter)
        s = jnp.einsum('bhld,bhsd->bhls', qb, kb) * jnp.exp(log_d - m_row[..., None])
        w_inter = jnp.exp(log_inter - m_row)
        num = (jnp.einsum('bhls,bhsd->bhld', s, vb)
               + w_inter[..., None] * jnp.einsum('bhvk,bhlk->bhlv', c_mem, qb))
        den = jnp.sum(s, axis=-1) + w_inter * jnp.einsum('bhk,bhlk->bhl', n_mem, qb)
        h = num / jnp.maximum(jnp.abs(den), jnp.exp(-m_row))[..., None]
        b_last = bcum[..., -1]
        log_w = b_last[..., None] - bcum + ib
        m_new = jnp.maximum(b_last + m_mem, jnp.max(log_w, axis=-1))
        w_upd = jnp.exp(log_w - m_new[..., None])
        carry_decay = jnp.exp(b_last + m_mem - m_new)
        c_new = carry_decay[..., None, None] * c_mem + jnp.einsum('bhl,bhlv,bhlk->bhvk', w_upd, vb, kb)
        n_new = carry_decay[..., None] * n_mem + jnp.einsum('bhl,bhlk->bhk', w_upd, kb)
        return (c_new, n_new, m_new), h

    init = (jnp.zeros((B, H, d, d), f32), jnp.zeros((B, H, d), f32), jnp.zeros((B, H), f32))
    _, h = lax.scan(step, init, (qc, kc, vc, ic, fc))
    return h.transpose(1, 0, 3, 2, 4).reshape(B, T, H, d)


def retention(q, k, v):
    B, T, _ = q.shape
    H, d, L = N_HEADS, HEAD_DIM, RET_CHUNK
    nc = T // L
    f32 = jnp.float32

    def heads(a):
        return a.astype(f32).reshape(B, nc, L, H, d).transpose(1, 0, 3, 2, 4)

    qc, kc, vc = heads(q), heads(k) * (d ** -0.5), heads(v)
    log_gamma = jnp.log(1.0 - 2.0 ** (-5.0 - jnp.arange(H, dtype=f32)))
    pos = jnp.arange(L, dtype=f32)
    diff = pos[:, None] - pos[None, :]
    decay = jnp.where(diff >= 0, jnp.exp(log_gamma[:, None, None] * jnp.maximum(diff, 0.0)), 0.0)
    q_decay = jnp.exp(log_gamma[:, None] * (pos + 1.0))[:, :, None]
    k_decay = jnp.exp(log_gamma[:, None] * (L - 1.0 - pos))[:, :, None]
    chunk_decay = jnp.exp(log_gamma * L)[:, None, None]

    def step(r_state, inp):
        qb, kb, vb = inp
        s = jnp.einsum('bhld,bhsd->bhls', qb, kb) * decay
        o = jnp.einsum('bhls,bhsv->bhlv', s, vb) + jnp.einsum('bhld,bhdv->bhlv', qb * q_decay, r_state)
        r_state = chunk_decay * r_state + jnp.einsum('bhsd,bhsv->bhdv', kb * k_decay, vb)
        return r_state, o

    _, o = lax.scan(step, jnp.zeros((B, H, d, d), f32), (qc, kc, vc))
    return o.transpose(1, 0, 3, 2, 4).reshape(B, T, H, d)


def nsa_attention(q, k_c, v_c, k_s, v_s, k_w, v_w, gates, cmp_w, cmp_pe):
    B, T, _ = q.shape
    f32 = jnp.float32
    G, J, d = NSA_KV_GROUPS, NSA_GROUP_SIZE, HEAD_DIM
    qh = q.astype(f32).reshape(B, T, G, J, d).transpose(0, 2, 3, 1, 4) * (d ** -0.5)

    def kv_heads(a):
        return a.astype(f32).reshape(B, T, G, d).transpose(0, 2, 1, 3)

    kc, vc, ks, vs, kw, vw = (kv_heads(a) for a in (k_c, v_c, k_s, v_s, k_w, v_w))
    slopes = alibi_slopes(N_HEADS).reshape(G, J)[None, :, :, None, None]
    t_pos = jnp.arange(T)
    nq = T // Q_BLOCK
    tq_b = t_pos.reshape(nq, Q_BLOCK)

    n_cmp = T // CMP_STRIDE - 1

    def compress(a, w, pe):
        ar = a.reshape(B, G, T // CMP_STRIDE, CMP_STRIDE, d)
        blocks = jnp.concatenate([ar[:, :, :-1], ar[:, :, 1:]], axis=3)
        return jnp.einsum('bgnld,lde->bgne', blocks + pe.astype(f32), w.astype(f32))

    k_cmp = compress(kc, cmp_w[0], cmp_pe[0])
    v_cmp = compress(vc, cmp_w[1], cmp_pe[1])
    cmp_end = jnp.arange(n_cmp) * CMP_STRIDE + CMP_BLOCK - 1
    dist_c = (t_pos[:, None] - cmp_end[None, :]).astype(f32)
    valid_c = dist_c >= 0
    s_c = jnp.einsum('bgjtd,bgnd->bgjtn', qh, k_cmp) - slopes * dist_c
    s_c = jnp.where(valid_c, s_c, NEG_INF)
    has_c = jnp.any(valid_c, axis=-1).astype(f32)[:, None]
    p_c = jax.nn.softmax(s_c, axis=-1) * has_c
    o_cmp = jnp.einsum('bgjtn,bgnd->bgjtd', p_c, v_cmp)

    n_sel = T // SEL_BLOCK
    topn = min(SEL_TOPN, n_sel)
    c_start = jnp.arange(n_cmp) * CMP_STRIDE
    s_start = jnp.arange(n_sel) * SEL_BLOCK
    overlap = ((c_start[:, None] < s_start[None, :] + SEL_BLOCK)
               & (c_start[:, None] + CMP_BLOCK > s_start[None, :])).astype(f32)
    importance = jnp.einsum('bgjtn,nm->bgtm', p_c, overlap)
    cur = t_pos // SEL_BLOCK
    blk = jnp.arange(n_sel)
    valid_s = blk[None, :] <= cur[:, None]
    forced = (blk[None, :] == 0) | (blk[None, :] == cur[:, None]) | (blk[None, :] == cur[:, None] - 1)
    score = jnp.where(valid_s, importance + jnp.where(forced, FORCE_SCORE, 0.0), NEG_INF)
    top_s, top_i = lax.top_k(score, topn)
    top_valid = top_s > 0.5 * NEG_INF

    ks_blk = ks.reshape(B, G, n_sel, SEL_BLOCK, d)
    vs_blk = vs.reshape(B, G, n_sel, SEL_BLOCK, d)
    q_b = qh.reshape(B, G, J, nq, Q_BLOCK, d).transpose(3, 0, 1, 2, 4, 5)
    i_b = top_i.reshape(B, G, nq, Q_BLOCK, topn).transpose(2, 0, 1, 3, 4)
    v_b = top_valid.reshape(B, G, nq, Q_BLOCK, topn).transpose(2, 0, 1, 3, 4)
    gather = jax.vmap(jax.vmap(lambda blocks, ix: blocks[ix]))
    within = jnp.arange(SEL_BLOCK)

    def sel_step(args):
        qb, ib, vb, tq = args
        kg = gather(ks_blk, ib)
        vg = gather(vs_blk, ib)
        kpos = ib[..., None] * SEL_BLOCK + within
        dist = (tq[None, None, :, None, None] - kpos).astype(f32)
        mask = vb[..., None] & (dist >= 0)
        s = jnp.einsum('bgjqd,bgqnsd->bgjqns', qb, kg) - slopes[..., None] * dist[:, :, None]
        s = jnp.where(mask[:, :, None], s, NEG_INF).reshape(B, G, J, Q_BLOCK, topn * SEL_BLOCK)
        p = jax.nn.softmax(s, axis=-1).reshape(B, G, J, Q_BLOCK, topn, SEL_BLOCK)
        return jnp.einsum('bgjqns,bgqnsd->bgjqd', p, vg)

    o_sel = lax.map(sel_step, (q_b, i_b, v_b, tq_b))
    o_sel = o_sel.transpose(1, 2, 3, 0, 4, 5).reshape(B, G, J, T, d)

    n_back = WINDOW // Q_BLOCK
    span = (n_back + 1) * Q_BLOCK

    def band(a):
        ap = jnp.pad(a, ((0, 0), (0, 0), (WINDOW, 0), (0, 0))).reshape(B, G, nq + n_back, Q_BLOCK, d)
        return jnp.concatenate([ap[:, :, i:i + nq] for i in range(n_back + 1)], axis=3)

    kwb, vwb = band(kw), band(vw)
    qw = qh.reshape(B, G, J, nq, Q_BLOCK, d)
    kpos_w = jnp.arange(nq)[:, None] * Q_BLOCK - WINDOW + jnp.arange(span)[None, :]
    dist_w = (tq_b[:, :, None] - kpos_w[:, None, :]).astype(f32)
    mask_w = (dist_w >= 0) & (dist_w < WINDOW) & (kpos_w[:, None, :] >= 0)
    s_w = jnp.einsum('bgjnqd,bgnkd->bgjnqk', qw, kwb) - slopes[..., None] * dist_w
    s_w = jnp.where(mask_w, s_w, NEG_INF)
    p_w = jax.nn.softmax(s_w, axis=-1)
    o_win = jnp.einsum('bgjnqk,bgnkd->bgjnqd', p_w, vwb).reshape(B, G, J, T, d)

    g = jax.nn.sigmoid(gates.astype(f32)).reshape(B, T, 3, G, J).transpose(2, 0, 3, 4, 1)[..., None]
    o = g[0] * o_cmp + g[1] * o_sel + g[2] * o_win
    return o.transpose(0, 3, 1, 2, 4).reshape(B, T, N_HEADS * d).astype(q.dtype)


def spatial_gating(u, v, norm_g, w_s, b_s):
    B, T, W = u.shape
    L, gr = SGU_CHUNK, N_HEADS
    nc = T // L
    u = jax.nn.gelu(u)
    v = rmsnorm(jax.nn.gelu(v), norm_g).reshape(B, nc, L, gr, W // gr)
    w_m = w_s * jnp.tril(jnp.ones((L, L), dtype=w_s.dtype))
    mixed = jnp.einsum('gts,bcsge->bctge', w_m, v) + b_s.T[:, :, None]
    return u * mixed.reshape(B, T, W)


def mixing_block(h, w_in, conv_w, gate_bias, mlstm_g, ret_g, cmp_w, cmp_pe,
                 sgu_g, sgu_w, sgu_b, w_branch, w_out):
    z = h @ w_in
    (aq, ak, av, ao, ai, af, bq, bk, bv, bg, cq, ckc, cvc, cks, cvs, ckw, cvw, cg,
     du, dv, merge_logits) = split_columns(z)
    qk = jax.nn.silu(causal_conv(jnp.concatenate([aq, ak], axis=-1), conv_w))
    aq2, ak2 = jnp.split(qk, 2, axis=-1)
    ha = mlstm(aq2, ak2, av, ai + gate_bias[0], af + gate_bias[1])
    ya = head_norm(ha, mlstm_g, h.dtype) * jax.nn.sigmoid(ao)
    hb = retention(bq, bk, bv)
    yb = head_norm(hb, ret_g, h.dtype) * jax.nn.silu(bg)
    yc = nsa_attention(cq, ckc, cvc, cks, cvs, ckw, cvw, cg, cmp_w, cmp_pe)
    yd = spatial_gating(du, dv, sgu_g, sgu_w, sgu_b)
    merged = None
    for n, y in enumerate((ya, yb, yc, yd)):
        term = jax.nn.sigmoid(merge_logits[..., n * D_MODEL:(n + 1) * D_MODEL]) * (y @ w_branch[n])
        merged = term if merged is None else merged + term
    return merged @ w_out


def hier_moe(h, wg, bg, we, be, w1, w3, w2):
    B, T, D = h.shape
    N = B * T
    K = TOP_K_IN_GROUP
    f32 = jnp.float32
    hf = h.reshape(N, D)
    grp_logits = (hf @ wg).astype(f32) + bg.astype(f32)
    grp_prob = jax.nn.softmax(grp_logits, axis=-1)
    g_idx = jnp.argmax(grp_logits, axis=-1).astype(jnp.int32)
    g_w = jnp.take_along_axis(grp_prob, g_idx[:, None], axis=-1)
    exp_logits = ((hf @ we).astype(f32) + be.astype(f32)).reshape(N, N_GROUPS, EXPERTS_PER_GROUP)
    in_grp = jnp.take_along_axis(exp_logits, g_idx[:, None, None], axis=1)[:, 0]
    top_v, top_i = lax.top_k(in_grp, K)
    comb = (jax.nn.softmax(top_v, axis=-1) * g_w).astype(h.dtype)
    e_idx = g_idx[:, None] * EXPERTS_PER_GROUP + top_i.astype(jnp.int32)

    nk = N * K
    flat_e = e_idx.reshape(-1)
    flat_tok = jnp.repeat(jnp.arange(N, dtype=jnp.int32), K)
    flat_w = comb.reshape(-1)
    order = jnp.argsort(flat_e)
    se, stok, sw = flat_e[order], flat_tok[order], flat_w[order]
    counts = jnp.bincount(flat_e, length=N_EXPERTS)
    padded = (counts + MOE_BLOCK - 1) // MOE_BLOCK * MOE_BLOCK
    pad_end = jnp.cumsum(padded)
    pad_start = pad_end - padded
    start = jnp.cumsum(counts) - counts
    dest = pad_start[se] + jnp.arange(nk, dtype=jnp.int32) - start[se]
    n_blk = (nk + MOE_BLOCK - 1) // MOE_BLOCK + N_EXPERTS
    P = n_blk * MOE_BLOCK
    tok_pad = jnp.full((P,), N, dtype=jnp.int32).at[dest].set(stok)
    w_pad = jnp.zeros((P,), dtype=h.dtype).at[dest].set(sw)
    blk_exp = jnp.minimum(jnp.searchsorted(pad_end, jnp.arange(n_blk) * MOE_BLOCK, side='right'),
                          N_EXPERTS - 1)
    h_ext = jnp.concatenate([hf, jnp.zeros((1, D), hf.dtype)], axis=0)

    def expert_block(args):
        tok, wt, e = args
        xb = h_ext[tok]
        a = jax.nn.silu(xb @ w1[e]) * (xb @ w3[e])
        return (a @ w2[e]) * wt[:, None]

    y = lax.map(expert_block, (tok_pad.reshape(n_blk, MOE_BLOCK), w_pad.reshape(n_blk, MOE_BLOCK), blk_exp))
    out = jax.ops.segment_sum(y.reshape(P, D), tok_pad, num_segments=N + 1)[:N]
    return out.reshape(B, T, D)


def setup_inputs(seed: int = 0) -> dict:
    key = jax.random.key(seed)
    ks = jax.random.split(key, 24)
    f32 = jnp.float32

    def nrm(k, shape, scale):
        return jax.random.normal(k, shape, f32) * scale

    def gain(k, shape):
        return 1.0 + 0.02 * jax.random.normal(k, shape, f32)

    f_bias = jnp.linspace(3.0, 6.0, N_HEADS, dtype=f32)
    return {
        "x": nrm(ks[0], (BATCH, SEQ, D_MODEL), 1.0),
        "w_in": nrm(ks[1], (DEPTH, D_MODEL, C_IN), D_MODEL ** -0.5),
        "mlstm_conv": nrm(ks[2], (DEPTH, CONV_WIDTH, 2 * MIX_WIDTH), CONV_WIDTH ** -0.5),
        "mlstm_gate_bias": jnp.stack([nrm(ks[3], (DEPTH, N_HEADS), 0.1),
                                      f_bias + nrm(ks[4], (DEPTH, N_HEADS), 0.1)], axis=1),
        "mlstm_norm": gain(ks[5], (DEPTH, MIX_WIDTH)),
        "ret_norm": gain(ks[6], (DEPTH, MIX_WIDTH)),
        "nsa_cmp_w": nrm(ks[7], (DEPTH, 2, CMP_BLOCK, HEAD_DIM, HEAD_DIM), (CMP_BLOCK * HEAD_DIM) ** -0.5),
        "nsa_cmp_pe": nrm(ks[8], (DEPTH, 2, CMP_BLOCK, HEAD_DIM), 0.1),
        "sgu_norm": gain(ks[9], (DEPTH, MIX_WIDTH)),
        "sgu_w": nrm(ks[10], (DEPTH, N_HEADS, SGU_CHUNK, SGU_CHUNK), SGU_CHUNK ** -0.5),
        "sgu_b": 1.0 + nrm(ks[11], (DEPTH, N_HEADS, SGU_CHUNK), 0.1),
        "w_branch": nrm(ks[12], (DEPTH, N_BRANCH, MIX_WIDTH, D_MODEL), MIX_WIDTH ** -0.5),
        "w_out": nrm(ks[13], (DEPTH, D_MODEL, D_MODEL), D_MODEL ** -0.5),
        "norm_mix": gain(ks[14], (DEPTH, D_MODEL)),
        "norm_ffn": gain(ks[15], (DEPTH, D_MODEL)),
        "router_group_w": nrm(ks[16], (DEPTH, D_MODEL, N_GROUPS), D_MODEL ** -0.5),
        "router_group_b": nrm(ks[17], (DEPTH, N_GROUPS), 0.01),
        "router_expert_w": nrm(ks[18], (DEPTH, D_MODEL, N_EXPERTS), D_MODEL ** -0.5),
        "router_expert_b": nrm(ks[19], (DEPTH, N_EXPERTS), 0.01),
        "expert_w1": nrm(ks[20], (DEPTH, N_EXPERTS, D_MODEL, EXPERT_FF), D_MODEL ** -0.5),
        "expert_w3": nrm(ks[21], (DEPTH, N_EXPERTS, D_MODEL, EXPERT_FF), D_MODEL ** -0.5),
        "expert_w2": nrm(ks[22], (DEPTH, N_EXPERTS, EXPERT_FF, D_MODEL), EXPERT_FF ** -0.5),
        "norm_final": gain(ks[23], (D_MODEL,)),
    }


def reference(x, w_in, mlstm_conv, mlstm_gate_bias, mlstm_norm, ret_norm, nsa_cmp_w, nsa_cmp_pe,
              sgu_norm, sgu_w, sgu_b, w_branch, w_out, norm_mix, norm_ffn, router_group_w,
              router_group_b, router_expert_w, router_expert_b, expert_w1, expert_w3, expert_w2,
              norm_final):
    for l in range(DEPTH):
        h = rmsnorm(x, norm_mix[l])
        x = x + mixing_block(h, w_in[l], mlstm_conv[l], mlstm_gate_bias[l], mlstm_norm[l], ret_norm[l],
                             nsa_cmp_w[l], nsa_cmp_pe[l], sgu_norm[l], sgu_w[l], sgu_b[l],
                             w_branch[l], w_out[l])
        h = rmsnorm(x, norm_ffn[l])
        x = x + hier_moe(h, router_group_w[l], router_group_b[l], router_expert_w[l], router_expert_b[l],
                         expert_w1[l], expert_w3[l], expert_w2[l])
    return rmsnorm(x, norm_final)
```

```python
import numpy as np
from contextlib import ExitStack
import concourse.bass as bass
import concourse.mybir as mybir
from concourse.bass_utils import run_bass_kernel_spmd

F32 = mybir.dt.float32
BF16 = mybir.dt.bfloat16
AF = mybir.ActivationFunctionType
ALU = mybir.AluOpType
AX = mybir.AxisListType


class TT:
    __slots__ = ("t", "w", "r", "name")

    def __init__(self, t, name=""):
        self.t = t
        self.w = None
        self.r = {}
        self.name = name

    def __getitem__(self, k):
        return self.t[k]

    def ap(self):
        return self.t.ap() if hasattr(self.t, "ap") else self.t[:]


class Res(TT):
    def __init__(self, name=""):
        TT.__init__(self, None, name)


class KB:
    NDMA = {"sp": 8, "pool": 6, "act": 4}

    def __init__(self, nc):
        self.nc = nc
        self.es = ExitStack()
        self.E = {"pe": nc.tensor, "dve": nc.vector, "act": nc.scalar, "pool": nc.gpsimd, "sp": nc.sync}
        self.sem = {}
        self.cnt = {}
        self.clk = {e: {} for e in self.E}
        for e in ("pe", "dve", "act", "pool"):
            self.sem[e] = self.es.enter_context(nc.semaphore("s_" + e))
            self.cnt[e] = 0
        self.dmai = {}
        for q, n in self.NDMA.items():
            self.dmai[q] = 0
            for i in range(n):
                k = (q, i)
                self.sem[k] = self.es.enter_context(nc.semaphore("d_%s%d" % (q, i)))
                self.cnt[k] = 0
        self.scopes = [self.es]
        self.nwait = 0
        self.nops = 0
        self._uid = 0

    def uid(self, p):
        self._uid += 1
        return "%s_%d" % (p, self._uid)

    def sb(self, name, shape, dt=F32):
        t = self.scopes[-1].enter_context(self.nc.sbuf_tensor(self.uid(name), list(shape), dt))
        return TT(t, name)

    def ps(self, name, shape, dt=F32):
        t = self.scopes[-1].enter_context(self.nc.psum_tensor(self.uid(name), list(shape), dt))
        return TT(t, name)

    def dram(self, name, shape, dt=F32, kind="Internal"):
        t = self.nc.dram_tensor(name, list(shape), dt, kind=kind)
        return TT(t, name)

    def push(self):
        s = ExitStack()
        self.scopes.append(s)
        return s

    def pop(self):
        self.barrier()
        s = self.scopes.pop()
        s.close()

    def _wait(self, eng, tok):
        key, val, snap = tok
        c = self.clk[eng]
        if c.get(key, 0) >= val:
            return
        self.E[eng].wait_ge(self.sem[key], val)
        self.nwait += 1
        for k2, v2 in snap.items():
            if c.get(k2, 0) < v2:
                c[k2] = v2
        c[key] = val

    def _deps(self, eng, r, w):
        deps = []
        for b in r:
            if b.w is not None:
                deps.append(b.w)
        for b in w:
            if b.w is not None:
                deps.append(b.w)
            deps.extend(b.r.values())
        for tok in deps:
            if eng == "pe" and tok[0] == "pe":
                continue
            self._wait(eng, tok)

    def _mark(self, tok, r, w):
        for b in r:
            b.r[tok[0]] = tok
        for b in w:
            b.w = tok
            b.r = {}

    def op(self, eng, fn, r=(), w=()):
        self._deps(eng, r, w)
        ins = fn(self.E[eng])
        self.cnt[eng] += 1
        ins.then_inc(self.sem[eng], 1)
        tok = (eng, self.cnt[eng], dict(self.clk[eng]))
        self._mark(tok, r, w)
        self.nops += 1
        return tok

    def dma(self, out, in_, r=(), w=(), q="sp", **kw):
        self._deps(q, r, w)
        i = self.dmai[q] % self.NDMA[q]
        self.dmai[q] += 1
        key = (q, i)
        if self.cnt[key] > 0:
            self._wait(q, (key, 16 * self.cnt[key], {}))
        ins = self.E[q].dma_start(out=out, in_=in_, **kw)
        self.cnt[key] += 1
        ins.then_inc(self.sem[key], 16)
        tok = (key, 16 * self.cnt[key], dict(self.clk[q]))
        self._mark(tok, r, w)
        self.nops += 1
        return tok

    def barrier(self):
        toks = []
        for k, s in self.sem.items():
            v = self.cnt[k] * (16 if isinstance(k, tuple) else 1)
            if v > 0:
                toks.append((k, v, {}))
        for e in self.E:
            for tok in toks:
                self._wait(e, tok)

    def mm(self, ps_ap, lhsT_ap, rhs_ap, start, stop, r, w):
        return self.op("pe", lambda e: e.matmul(ps_ap, lhsT_ap, rhs_ap, start=start, stop=stop), r=r, w=w)

    def transpose(self, ps_ap, in_ap, ident_ap, r, w):
        return self.op("pe", lambda e: e.transpose(ps_ap, in_ap, ident_ap), r=r, w=w)

    def act(self, out, in_, func, r, w, eng="act", **kw):
        return self.op(eng, lambda e: e.activation(out=out, in_=in_, func=func, **kw), r=r, w=w)

    def copy(self, eng, out, in_, r, w):
        if eng == "act":
            return self.op(eng, lambda e: e.copy(out=out, in_=in_), r=r, w=w)
        return self.op(eng, lambda e: e.tensor_copy(out=out, in_=in_), r=r, w=w)

    def tt(self, eng, out, in0, in1, op, r, w):
        return self.op(eng, lambda e: e.tensor_tensor(out=out, in0=in0, in1=in1, op=op), r=r, w=w)

    def ts(self, eng, out, in0, s1, op0, r, w, s2=None, op1=None, **kw):
        if op1 is None:
            return self.op(eng, lambda e: e.tensor_scalar(out=out, in0=in0, scalar1=s1, scalar2=None, op0=op0, **kw), r=r, w=w)
        return self.op(eng, lambda e: e.tensor_scalar(out=out, in0=in0, scalar1=s1, scalar2=s2, op0=op0, op1=op1, **kw), r=r, w=w)

    def stt(self, out, in0, scalar, in1, op0, op1, r, w, **kw):
        return self.op("dve", lambda e: e.scalar_tensor_tensor(out=out, in0=in0, scalar=scalar, in1=in1, op0=op0, op1=op1, **kw), r=r, w=w)


class Rot:
    def __init__(self, kb, name, shape, dt, n, kind="sb"):
        mk = kb.sb if kind == "sb" else kb.ps
        self.bufs = [mk("%s%d" % (name, i), shape, dt) for i in range(n)]
        self.i = 0

    def next(self):
        b = self.bufs[self.i % len(self.bufs)]
        self.i += 1
        return b


RMS_EPS = 1e-6


def phase_norm(kb, xT, gcol, hT, D, T, TT_=512):
    nc = kb.nc
    KC = D // 128
    kb.push()
    g_sb = kb.sb("g", [128, KC])
    kb.dma(g_sb[:], gcol.ap(), r=[gcol], w=[g_sb])
    ones = kb.sb("ones", [128, 128])
    kb.op("dve", lambda e: e.memset(ones[:], 1.0), w=[ones])
    xt = kb.sb("xt", [128, KC, TT_])
    sq = Rot(kb, "sq", [128, TT_], F32, 3)
    ssp = Rot(kb, "ssp", [128, TT_], F32, 2, kind="ps")
    rstd = kb.sb("rstd", [128, TT_])
    hb = Rot(kb, "hb", [128, KC, TT_], BF16, 2)
    xv = xT.ap().rearrange("(c p) t -> p c t", p=128)
    hv = hT.ap().rearrange("(c p) t -> p c t", p=128)
    NP = max(1, KC // 8)
    for ti in range(T // TT_):
        t0 = ti * TT_
        for j in range(NP):
            c0, c1 = j * KC // NP, (j + 1) * KC // NP
            kb.dma(xt[:, c0:c1, :], xv[:, c0:c1, t0:t0 + TT_], r=[xT], w=[xt], q=("sp" if j % 2 == 0 else "pool"))
        ss = ssp.next()
        for kc in range(KC):
            s = sq.next()
            kb.act(s[:], xt[:, kc, :], AF.Square, r=[xt], w=[s])
            kb.mm(ss[:], ones[:], s[:], kc == 0, kc == KC - 1, r=[ones, s], w=[ss])
        kb.ts("dve", rstd[:], ss[:], 1.0 / D, ALU.mult, r=[ss], w=[rstd], s2=RMS_EPS, op1=ALU.add)
        kb.act(rstd[:], rstd[:], AF.Sqrt, r=[rstd], w=[rstd])
        kb.op("dve", lambda e: e.reciprocal(out=rstd[:], in_=rstd[:]), r=[rstd], w=[rstd])
        h = hb.next()
        for kc in range(KC):
            eng = "dve"
            kb.stt(h[:, kc, :], xt[:, kc, :], g_sb[:, kc:kc + 1], rstd[:], ALU.mult, ALU.mult, r=[xt, g_sb, rstd], w=[h])
        for j in range(NP):
            c0, c1 = j * KC // NP, (j + 1) * KC // NP
            kb.dma(hv[:, c0:c1, t0:t0 + TT_], h[:, c0:c1, :], r=[h], w=[hT], q=("sp" if j % 2 == 0 else "pool"))
    kb.pop()


def phase_proj(kb, hT, W, groups, D, T, TT_=512):
    KC = D // 128
    kb.push()
    wb = kb.sb("wb", [128, KC, 1024], BF16)
    wst = Rot(kb, "wst", [128, 4, 1024], F32, 2)
    hts = Rot(kb, "hts", [128, KC, TT_], BF16, 2)
    psr = Rot(kb, "pp", [128, 512], F32, 4, kind="ps")
    ost = Rot(kb, "ost", [128, 1024], F32, 3)
    Wv = W.ap().rearrange("(c p) n -> p c n", p=128)
    hv = hT.ap().rearrange("(c p) t -> p c t", p=128)
    NP = max(1, KC // 8)
    evi = [0]

    def evac(dst_ap, src_ap, r, w):
        evi[0] += 1
        kb.copy("act" if evi[0] % 2 == 0 else "dve", dst_ap, src_ap, r=r, w=w)

    pieces = []
    for (c0, width, kind, dst, doff) in groups:
        o = 0
        while o < width:
            wpc = min(1024, width - o)
            pieces.append((c0 + o, wpc, kind, dst, doff + o))
            o += wpc
    for (c0, wd, kind, dst, doff) in pieces:
        for k4 in range(0, KC, 4):
            st = wst.next()
            kb.dma(st[:, :, :wd], Wv[:, k4:k4 + 4, c0:c0 + wd], r=[W], w=[st], q=("sp" if (k4 // 4) % 2 == 0 else "pool"))
            evac(wb[:, k4:k4 + 4, :wd], st[:, :, :wd], [st], [wb])
        for ti in range(T // TT_):
            t0 = ti * TT_
            ht = hts.next()
            for j in range(NP):
                a, b = j * KC // NP, (j + 1) * KC // NP
                kb.dma(ht[:, a:b, :], hv[:, a:b, t0:t0 + TT_], r=[hT], w=[ht], q=("sp" if j % 2 == 0 else "pool"))
            if kind == "F":
                for m0 in range(0, wd, 128):
                    M = min(128, wd - m0)
                    ps = psr.next()
                    for kc in range(KC):
                        kb.mm(ps[:M, :TT_], wb[:, kc, m0:m0 + M], ht[:, kc, :], kc == 0, kc == KC - 1, r=[wb, ht], w=[ps])
                    o = ost.next()
                    evac(o[:M, :TT_], ps[:M, :TT_], [ps], [o])
                    kb.dma(dst.ap()[doff + m0:doff + m0 + M, t0:t0 + TT_], o[:M, :TT_], r=[o], w=[dst])
            else:
                for sub in range(TT_ // 128):
                    o = ost.next()
                    for n0 in range(0, wd, 512):
                        N = min(512, wd - n0)
                        ps = psr.next()
                        for kc in range(KC):
                            kb.mm(ps[:, :N], ht[:, kc, sub * 128:(sub + 1) * 128], wb[:, kc, n0:n0 + N], kc == 0, kc == KC - 1, r=[wb, ht], w=[ps])
                        evac(o[:, n0:n0 + N], ps[:, :N], [ps], [o])
                    kb.dma(dst.ap()[t0 + sub * 128:t0 + (sub + 1) * 128, doff:doff + wd], o[:, :wd], r=[o], w=[dst])
    kb.pop()


HD = 128
EPS = 1e-6
BIG = 1.0e30


def conv_silu(kb, zF, row0, convw_sb, wi, dst, T, scale, rawrot, tmprot):
    PW = min(1024, T)
    for p0 in range(0, T, PW):
        raw = rawrot.next()
        if p0 == 0:
            kb.op("pool", lambda e: e.memset(raw[:, 0:3], 0.0), w=[raw])
            kb.dma(raw[:, 3:3 + PW], zF.ap()[row0:row0 + 128, 0:PW], r=[zF], w=[raw])
        else:
            kb.dma(raw[:, 0:3 + PW], zF.ap()[row0:row0 + 128, p0 - 3:p0 + PW], r=[zF], w=[raw])
        tmp = tmprot.next()
        kb.ts("dve", tmp[:, :PW], raw[:, 0:PW], convw_sb[:, wi, 0:1], ALU.mult, r=[raw, convw_sb], w=[tmp])
        for j in range(1, 4):
            kb.stt(tmp[:, :PW], raw[:, j:j + PW], convw_sb[:, wi, j:j + 1], tmp[:, :PW], ALU.mult, ALU.add, r=[raw, convw_sb, tmp], w=[tmp])
        if scale == 1.0:
            kb.act(dst[:, p0:p0 + PW], tmp[:, :PW], AF.Silu, r=[tmp], w=[dst])
        else:
            kb.act(tmp[:, :PW], tmp[:, :PW], AF.Silu, r=[tmp], w=[tmp])
            kb.ts("dve", dst[:, p0:p0 + PW], tmp[:, :PW], scale, ALU.mult, r=[tmp], w=[dst])


def head_out(kb, h_ap, h_res, ssq, rs, g_ap, g_res, gate_ap, gate_res, gate_func, y_ap, y_res, junk):
    kb.act(junk[:], h_ap, AF.Square, r=[h_res], w=[junk, ssq], accum_out=ssq[:])
    kb.ts("dve", rs[:], ssq[:], 1.0 / HD, ALU.mult, r=[ssq], w=[rs], s2=EPS, op1=ALU.add)
    kb.act(rs[:], rs[:], AF.Sqrt, r=[rs], w=[rs])
    kb.op("dve", lambda e: e.reciprocal(out=rs[:], in_=rs[:]), r=[rs], w=[rs])
    kb.act(junk[:], gate_ap, gate_func, r=[gate_res], w=[junk])
    kb.stt(y_ap, h_ap, rs[:, 0:1], g_ap, ALU.mult, ALU.mult, r=[h_res, rs, g_res], w=[y_res])
    kb.tt("dve", y_ap, y_ap, junk[:], ALU.mult, r=[y_res, junk], w=[y_res])


def mixer_mlstm(kb, zF_qk, zF_g, zT_vo, convw, gbias, gnorm, cst, y, ycol0, T, scr):
    NCH = T // 128
    kb.push()
    ident = kb.sb("ident", [128, 128]); kb.dma(ident[:], cst["ident"].ap(), r=[cst["ident"]], w=[ident])
    bigm = kb.sb("bigm", [128, 128]); kb.dma(bigm[:], cst["bigmaskT"].ap(), r=[cst["bigmaskT"]], w=[bigm])
    cw = kb.sb("cw", [128, 4, 4]); kb.dma(cw[:], convw.ap(), r=[convw], w=[cw])
    gb = kb.sb("gb", [64, 4]); kb.dma(gb[:], gbias.ap(), r=[gbias], w=[gb])
    gn = kb.sb("gn", [128, 256]); kb.dma(gn[:], gnorm.ap(), r=[gnorm], w=[gn])
    ones = kb.sb("ones", [128, 128]); kb.op("dve", lambda e: e.memset(ones[:], 1.0), w=[ones])
    G4 = kb.sb("G4", [NCH, 4, 128])
    kb.dma(G4[:], zF_g.ap().rearrange("r (c l) -> c r l", l=128), r=[zF_g], w=[G4])
    qT = kb.sb("qT", [128, T]); kT = kb.sb("kT", [128, T])
    Mbc = kb.sb("Mbc", [128, T])
    rawrot = Rot(kb, "raw", [128, 3 + min(1024, T)], F32, 2)
    tmprot = Rot(kb, "ctmp", [128, min(1024, T)], F32, 2)
    psr = Rot(kb, "ps", [128, 512], F32, 6, kind="ps")
    def ct(name, w=128):
        return kb.sb(name, [NCH, w])
    ib, fb, bc, a, cm, M, wi_, en, wu = [ct(n) for n in ("ib", "fb", "bc", "a", "cm", "M", "wi", "en", "wu")]
    sm = kb.sb("sm", [NCH, 8])
    rowA = kb.sb("rowA", [1, NCH]); rowB = kb.sb("rowB", [1, NCH]); rowM = kb.sb("rowM", [1, NCH + 1])
    cols = kb.sb("cols", [128, 4, NCH])
    dbc = kb.sb("dbc", [128, NCH])
    ddiag = kb.sb("ddiag", [NCH, NCH])
    CT = kb.sb("CT", [128, 132])
    vrot = Rot(kb, "v", [128, 132], F32, 3); orot = Rot(kb, "o", [128, 128], F32, 3)
    Erot = Rot(kb, "E", [128, 128], F32, 2); STrot = Rot(kb, "ST", [128, 128], F32, 2)
    t1rot = Rot(kb, "t1", [128, 132], F32, 2); totrot = Rot(kb, "tot", [128, 132], F32, 2)
    ktrot = Rot(kb, "kt", [128, 128], F32, 2); vwrot = Rot(kb, "vw", [128, 132], F32, 2)
    yrot = Rot(kb, "y", [128, 128], F32, 3); junk = kb.sb("junk", [128, 128])
    ssq = kb.sb("ssq", [128, 1]); rs = kb.sb("rs", [128, 1]); dn = kb.sb("dn", [128, 1])
    VW = 129
    for hh in range(2):
        kb.ts("dve", ib[:], G4[:, hh, :], gb[:NCH, hh:hh + 1], ALU.add, r=[G4, gb], w=[ib])
        kb.ts("dve", sm[:, 6:7], gb[:NCH, 2 + hh:3 + hh], -1.0, ALU.mult, r=[gb], w=[sm])
        kb.act(fb[:], G4[:, 2 + hh, :], AF.Exp, r=[G4, sm], w=[fb], scale=-1.0, bias=sm[:, 6:7])
        kb.act(fb[:], fb[:], AF.Ln, r=[fb], w=[fb], bias=1.0)
        kb.ts("dve", fb[:], fb[:], -1.0, ALU.mult, r=[fb], w=[fb])
        kb.op("dve", lambda e: e.tensor_tensor_scan(out=bc[:], data0=ones[:NCH, :], data1=fb[:], initial=0.0, op0=ALU.mult, op1=ALU.add), r=[ones, fb], w=[bc])
        kb.tt("dve", a[:], ib[:], bc[:], ALU.subtract, r=[ib, bc], w=[a])
        kb.op("dve", lambda e: e.tensor_tensor_scan(out=cm[:], data0=ones[:NCH, :], data1=a[:], initial=-BIG, op0=ALU.mult, op1=ALU.max), r=[ones, a], w=[cm])
        kb.copy("dve", sm[:, 0:1], bc[:, 127:128], r=[bc], w=[sm])
        kb.tt("dve", sm[:, 1:2], bc[:, 127:128], cm[:, 127:128], ALU.add, r=[bc, cm], w=[sm])
        p = psr.next()
        kb.transpose(p[0:1, 0:NCH], sm[:, 0:1], ident[:NCH, :NCH], r=[sm, ident], w=[p])
        kb.copy("dve", rowA[:], p[0:1, 0:NCH], r=[p], w=[rowA])
        p = psr.next()
        kb.transpose(p[0:1, 0:NCH], sm[:, 1:2], ident[:NCH, :NCH], r=[sm, ident], w=[p])
        kb.copy("dve", rowB[:], p[0:1, 0:NCH], r=[p], w=[rowB])
        kb.op("dve", lambda e: e.memset(rowM[:, 0:1], 0.0), w=[rowM])
        kb.op("dve", lambda e: e.tensor_tensor_scan(out=rowM[:, 1:NCH + 1], data0=rowA[:], data1=rowB[:], initial=0.0, op0=ALU.add, op1=ALU.max), r=[rowA, rowB], w=[rowM])
        p = psr.next()
        kb.transpose(p[0:NCH, 0:1], rowM[:, 0:NCH], ident[0:1, 0:1], r=[rowM, ident], w=[p])
        kb.copy("dve", sm[:, 2:3], p[0:NCH, 0:1], r=[p], w=[sm])
        p = psr.next()
        kb.transpose(p[0:NCH, 0:1], rowM[:, 1:NCH + 1], ident[0:1, 0:1], r=[rowM, ident], w=[p])
        kb.copy("dve", sm[:, 3:4], p[0:NCH, 0:1], r=[p], w=[sm])
        kb.tt("dve", sm[:, 4:5], sm[:, 0:1], sm[:, 3:4], ALU.subtract, r=[sm], w=[sm])
        kb.tt("dve", sm[:, 5:6], sm[:, 4:5], sm[:, 2:3], ALU.add, r=[sm], w=[sm])
        kb.act(sm[:, 5:6], sm[:, 5:6], AF.Exp, r=[sm], w=[sm])
        kb.ts("dve", M[:], cm[:], sm[:, 2:3], ALU.max, r=[cm, sm], w=[M])
        kb.act(wi_[:], M[:], AF.Exp, r=[M, sm], w=[wi_], scale=-1.0, bias=sm[:, 2:3])
        kb.tt("dve", en[:], bc[:], M[:], ALU.add, r=[bc, M], w=[en])
        kb.act(en[:], en[:], AF.Exp, r=[en], w=[en], scale=-1.0)
        kb.act(wu[:], a[:], AF.Exp, r=[a, sm], w=[wu], bias=sm[:, 4:5])
        for i, src in enumerate((a, wi_, en, wu)):
            p = psr.next()
            kb.transpose(p[:, 0:NCH], src[:], ident[:NCH, :NCH], r=[src, ident], w=[p])
            kb.copy("dve", cols[:, i, :], p[:, 0:NCH], r=[p], w=[cols])
        kb.ts("dve", ddiag[:], ident[:NCH, :NCH], sm[:, 5:6], ALU.mult, r=[ident, sm], w=[ddiag])
        p = psr.next()
        kb.mm(p[:, 0:NCH], ones[:NCH, :], ddiag[:], True, True, r=[ones, ddiag], w=[p])
        kb.copy("dve", dbc[:], p[:, 0:NCH], r=[p], w=[dbc])
        kb.dma(scr.ap()[hh, :].rearrange("(c l) -> c l", l=128), M[:], r=[M], w=[scr])
        kb.dma(Mbc[:], scr.ap()[hh:hh + 1, :].broadcast_to([128, T]), r=[scr], w=[Mbc])
        Mv = Mbc[:].rearrange("p (c l) -> p c l", l=128)
        kb.tt("dve", Mv, Mv, bigm[:].unsqueeze(1).broadcast_to([128, NCH, 128]), ALU.add, r=[Mbc, bigm], w=[Mbc])
        conv_silu(kb, zF_qk, hh * 128, cw, hh, qT, T, HD ** -0.5, rawrot, tmprot)
        conv_silu(kb, zF_qk, 256 + hh * 128, cw, 2 + hh, kT, T, 1.0, rawrot, tmprot)
        kb.op("dve", lambda e: e.memset(CT[:], 0.0), w=[CT])
        for c in range(NCH):
            t0 = c * 128
            v = vrot.next(); o = orot.next()
            kb.dma(v[:, 0:128], zT_vo.ap()[t0:t0 + 128, hh * 128:(hh + 1) * 128], r=[zT_vo], w=[v], q="pool")
            kb.op("pool", lambda e: e.memset(v[:, 128:129], 1.0), w=[v])
            kb.dma(o[:], zT_vo.ap()[t0:t0 + 128, 256 + hh * 128:256 + (hh + 1) * 128], r=[zT_vo], w=[o], q="pool")
            ps_s = psr.next()
            kb.mm(ps_s[:, 0:128], kT[:, t0:t0 + 128], qT[:, t0:t0 + 128], True, True, r=[kT, qT], w=[ps_s])
            E = Erot.next()
            kb.act(E[:], Mbc[:, t0:t0 + 128], AF.Exp, r=[Mbc, cols], w=[E], scale=-1.0, bias=cols[:, 0, c:c + 1])
            ST = STrot.next()
            kb.tt("dve", ST[:], ps_s[:, 0:128], E[:], ALU.mult, r=[ps_s, E], w=[ST])
            ps_n = psr.next()
            kb.mm(ps_n[:, 0:VW], ST[:], v[:, 0:VW], True, True, r=[ST, v], w=[ps_n])
            ps_i = psr.next()
            kb.mm(ps_i[:, 0:VW], qT[:, t0:t0 + 128], CT[:, 0:VW], True, True, r=[qT, CT], w=[ps_i])
            t1 = t1rot.next()
            kb.copy("act", t1[:, 0:VW], ps_n[:, 0:VW], r=[ps_n], w=[t1])
            tot = totrot.next()
            kb.stt(tot[:, 0:VW], ps_i[:, 0:VW], cols[:, 1, c:c + 1], t1[:, 0:VW], ALU.mult, ALU.add, r=[ps_i, cols, t1], w=[tot])
            kb.ts("dve", dn[:], tot[:, 128:129], -1.0, ALU.mult, r=[tot], w=[dn])
            kb.tt("dve", dn[:], dn[:], tot[:, 128:129], ALU.max, r=[dn, tot], w=[dn])
            kb.ts("dve", dn[:], dn[:], cols[:, 2, c:c + 1], ALU.max, r=[dn, cols], w=[dn])
            kb.op("dve", lambda e: e.reciprocal(out=dn[:], in_=dn[:]), r=[dn], w=[dn])
            kb.ts("dve", tot[:, 0:128], tot[:, 0:128], dn[:, 0:1], ALU.mult, r=[tot, dn], w=[tot])
            yt = yrot.next()
            head_out(kb, tot[:, 0:128], tot, ssq, rs, gn[:, hh * 128:(hh + 1) * 128], gn, o[:], o, AF.Sigmoid, yt[:], yt, junk)
            kb.dma(y.ap()[t0:t0 + 128, ycol0 + hh * 128:ycol0 + (hh + 1) * 128], yt[:], r=[yt], w=[y])
            ps_k = psr.next()
            kb.transpose(ps_k[:, 0:128], kT[:, t0:t0 + 128], ident[:], r=[kT, ident], w=[ps_k])
            kt = ktrot.next()
            kb.copy("act", kt[:], ps_k[:, 0:128], r=[ps_k], w=[kt])
            vw = vwrot.next()
            kb.ts("dve", vw[:, 0:VW], v[:, 0:VW], cols[:, 3, c:c + 1], ALU.mult, r=[v, cols], w=[vw])
            ps_u = psr.next()
            kb.mm(ps_u[:, 0:VW], kt[:], vw[:, 0:VW], True, True, r=[kt, vw], w=[ps_u])
            kb.stt(CT[:, 0:VW], CT[:, 0:VW], dbc[:, c:c + 1], ps_u[:, 0:VW], ALU.mult, ALU.add, r=[CT, dbc, ps_u], w=[CT])
    kb.pop()


def mixer_ret(kb, zF_qk, zT_vg, gnorm, cst, rdec, rcols, y, ycol0, T):
    NCH = T // 128
    kb.push()
    ident = kb.sb("ident", [128, 128]); kb.dma(ident[:], cst["ident"].ap(), r=[cst["ident"]], w=[ident])
    gn = kb.sb("gn", [128, 256]); kb.dma(gn[:], gnorm.ap(), r=[gnorm], w=[gn])
    dec = kb.sb("dec", [128, 2, 128]); kb.dma(dec[:], rdec.ap(), r=[rdec], w=[dec])
    rc = kb.sb("rc", [128, 2, 3]); kb.dma(rc[:], rcols.ap(), r=[rcols], w=[rc])
    psr = Rot(kb, "ps", [128, 512], F32, 6, kind="ps")
    R = kb.sb("R", [128, 128])
    qrot = Rot(kb, "q", [128, 128], F32, 3); krot = Rot(kb, "k", [128, 128], F32, 3)
    vrot = Rot(kb, "v", [128, 128], F32, 3); orot = Rot(kb, "o", [128, 128], F32, 3)
    STrot = Rot(kb, "ST", [128, 128], F32, 2)
    t1rot = Rot(kb, "t1", [128, 128], F32, 2); totrot = Rot(kb, "tot", [128, 128], F32, 2)
    ktrot = Rot(kb, "kt", [128, 128], F32, 2); vwrot = Rot(kb, "vw", [128, 128], F32, 2)
    yrot = Rot(kb, "y", [128, 128], F32, 3); junk = kb.sb("junk", [128, 128])
    ssq = kb.sb("ssq", [128, 1]); rs = kb.sb("rs", [128, 1])
    for hh in range(2):
        kb.op("dve", lambda e: e.memset(R[:], 0.0), w=[R])
        for c in range(NCH):
            t0 = c * 128
            q = qrot.next(); k = krot.next(); v = vrot.next(); o = orot.next()
            kb.dma(q[:], zF_qk.ap()[hh * 128:(hh + 1) * 128, t0:t0 + 128], r=[zF_qk], w=[q])
            kb.dma(k[:], zF_qk.ap()[256 + hh * 128:256 + (hh + 1) * 128, t0:t0 + 128], r=[zF_qk], w=[k])
            kb.dma(v[:], zT_vg.ap()[t0:t0 + 128, hh * 128:(hh + 1) * 128], r=[zT_vg], w=[v], q="pool")
            kb.dma(o[:], zT_vg.ap()[t0:t0 + 128, 256 + hh * 128:256 + (hh + 1) * 128], r=[zT_vg], w=[o], q="pool")
            ps_s = psr.next()
            kb.mm(ps_s[:, 0:128], k[:], q[:], True, True, r=[k, q], w=[ps_s])
            ST = STrot.next()
            kb.tt("dve", ST[:], ps_s[:, 0:128], dec[:, hh, :], ALU.mult, r=[ps_s, dec], w=[ST])
            ps_n = psr.next()
            kb.mm(ps_n[:, 0:128], ST[:], v[:], True, True, r=[ST, v], w=[ps_n])
            ps_i = psr.next()
            kb.mm(ps_i[:, 0:128], q[:], R[:], True, True, r=[q, R], w=[ps_i])
            t1 = t1rot.next()
            kb.copy("act", t1[:], ps_n[:, 0:128], r=[ps_n], w=[t1])
            tot = totrot.next()
            kb.stt(tot[:], ps_i[:, 0:128], rc[:, hh, 0:1], t1[:], ALU.mult, ALU.add, r=[ps_i, rc, t1], w=[tot])
            yt = yrot.next()
            head_out(kb, tot[:], tot, ssq, rs, gn[:, hh * 128:(hh + 1) * 128], gn, o[:], o, AF.Silu, yt[:], yt, junk)
            kb.dma(y.ap()[t0:t0 + 128, ycol0 + hh * 128:ycol0 + (hh + 1) * 128], yt[:], r=[yt], w=[y])
            ps_k = psr.next()
            kb.transpose(ps_k[:, 0:128], k[:], ident[:], r=[k, ident], w=[ps_k])
            kt = ktrot.next()
            kb.copy("act", kt[:], ps_k[:, 0:128], r=[ps_k], w=[kt])
            vw = vwrot.next()
            kb.ts("dve", vw[:], v[:], rc[:, hh, 1:2], ALU.mult, r=[v, rc], w=[vw])
            ps_u = psr.next()
            kb.mm(ps_u[:, 0:128], kt[:], vw[:], True, True, r=[kt, vw], w=[ps_u])
            kb.stt(R[:], R[:], rc[:, hh, 2:3], ps_u[:, 0:128], ALU.mult, ALU.add, r=[R, rc, ps_u], w=[R])
    kb.pop()


NEG = -1.0e30


def mixer_nsa(kb, zF_C, zT_C, cmpw, cmppe, tabs, cst, y, ycol0, T):
    NQ = T // 128
    NCMP = T // 16 - 1
    NSEL = T // 64
    NCT = (NCMP + 127) // 128
    kb.push()
    ident = kb.sb("ident", [128, 128]); kb.dma(ident[:], cst["ident"].ap(), r=[cst["ident"]], w=[ident])
    kcmpT = kb.sb("kcmpT", [128, 512])
    vcmp = kb.sb("vcmp", [128, 4, 128])
    psr = Rot(kb, "ps", [128, 512], F32, 7, kind="ps")
    kb.push()
    cw = kb.sb("cw", [128, 2, 32, 128])
    for kv in range(2):
        kb.dma(cw[:, kv, :, :], cmpw.ap()[:, kv, :, :], r=[cmpw], w=[cw], q=("sp" if kv == 0 else "pool"))
    pe = kb.sb("pe", [128, 2, 32]); kb.dma(pe[:], cmppe.ap(), r=[cmppe], w=[pe])
    kcT = kb.sb("kcT", [128, T]); vcT = kb.sb("vcT", [128, T])
    kb.dma(kcT[:], zF_C.ap()[512:640, :], r=[zF_C], w=[kcT])
    kb.dma(vcT[:], zF_C.ap()[640:768, :], r=[zF_C], w=[vcT], q="pool")
    xl = Rot(kb, "xl", [128, 512], F32, 3)
    kv16 = kcT[:].rearrange("p (n s) -> p n s", s=16)
    vv16 = vcT[:].rearrange("p (n s) -> p n s", s=16)
    ps_k = psr.next()
    for l in range(32):
        x = xl.next()
        src = kv16[:, 0:NCMP, l] if l < 16 else kv16[:, 1:NCMP + 1, l - 16]
        kb.ts("dve", x[:, 0:NCMP], src, pe[:, 0, l:l + 1], ALU.add, r=[kcT, pe], w=[x])
        kb.mm(ps_k[:, 0:NCMP], cw[:, 0, l, :], x[:, 0:NCMP], l == 0, l == 31, r=[cw, x], w=[ps_k])
    kb.copy("act", kcmpT[:, 0:NCMP], ps_k[:, 0:NCMP], r=[ps_k], w=[kcmpT])
    ps_v = [psr.next() for _ in range(NCT)]
    for l in range(32):
        x = xl.next()
        src = vv16[:, 0:NCMP, l] if l < 16 else vv16[:, 1:NCMP + 1, l - 16]
        kb.ts("dve", x[:, 0:NCMP], src, pe[:, 1, l:l + 1], ALU.add, r=[vcT, pe], w=[x])
        for nt in range(NCT):
            nn = min(128, NCMP - nt * 128)
            kb.mm(ps_v[nt][:nn, 0:128], x[:, nt * 128:nt * 128 + nn], cw[:, 1, l, :], l == 0, l == 31, r=[cw, x], w=[ps_v[nt]])
    kb.op("dve", lambda e: e.memset(vcmp[:], 0.0), w=[vcmp])
    for nt in range(NCT):
        nn = min(128, NCMP - nt * 128)
        kb.copy("act", vcmp[:nn, nt, :], ps_v[nt][:nn, 0:128], r=[ps_v[nt]], w=[vcmp])
    kb.pop()
    kb.push()
    CB = kb.sb("CB", [128, 4, 520]); kb.dma(CB[:], tabs["CB"].ap(), r=[tabs["CB"]], w=[CB])
    RS = kb.sb("RS", [128, 2, 512]); kb.dma(RS[:], tabs["RS"].ap(), r=[tabs["RS"]], w=[RS])
    RD = kb.sb("RD", [128, 2, 128]); kb.dma(RD[:], tabs["RD"].ap(), r=[tabs["RD"]], w=[RD])
    WB = kb.sb("WB", [128, 2, 640]); kb.dma(WB[:], tabs["WB"].ap(), r=[tabs["WB"]], w=[WB])
    SL = kb.sb("SL", [128, 2, 64]); kb.dma(SL[:], tabs["SL"].ap(), r=[tabs["SL"]], w=[SL])
    CM = kb.sb("CM", [128, 255]); kb.dma(CM[:], tabs["CM"].ap(), r=[tabs["CM"]], w=[CM])
    OV = kb.sb("OV", [128, 4, 128]); kb.dma(OV[:], tabs["OV"].ap(), r=[tabs["OV"]], w=[OV])
    ksT = kb.sb("ksT", [128, T]); kb.dma(ksT[:], zF_C.ap()[768:896, :], r=[zF_C], w=[ksT])
    vs = kb.sb("vs", [128, NQ, 128])
    kb.dma(vs[:], zT_C.ap()[:, 0:128].rearrange("(c p) d -> p c d", p=128), r=[zT_C], w=[vs], q="pool")
    sc = kb.sb("sc", [128, T])
    qrot = Rot(kb, "q", [128, 4, 128], F32, 2)
    grot = Rot(kb, "gt", [128, 6], F32, 2)
    kwrot = Rot(kb, "kw", [128, 640], F32, 2)
    vwrot = Rot(kb, "vw", [128, 5, 128], F32, 2)
    scc = Rot(kb, "scc", [128, 512], F32, 2)
    prot = Rot(kb, "p", [128, 512], F32, 2)
    pT = kb.sb("pT", [128, 4, 4, 128])
    eT = Rot(kb, "eT", [128, 512], F32, 3)
    scw = Rot(kb, "scw", [128, 640], F32, 2)
    sco = kb.sb("sco", [128, 128]); sco2 = kb.sb("sco2", [128, 128])
    m8a = kb.sb("m8a", [128, 8]); m8b = kb.sb("m8b", [128, 8])
    nm = kb.sb("nm", [128, 128])
    st = Rot(kb, "st", [128, 8], F32, 4)
    acc = Rot(kb, "acc", [128, 256], F32, 2)
    evi = [0]

    def evac(dst_ap, src_ap, r, w):
        evi[0] += 1
        kb.copy("act" if evi[0] % 2 == 0 else "dve", dst_ap, src_ap, r=r, w=w)

    def softmax_stats(s_, row_ap, row_res, out_ap, out_res, clamp):
        kb.op("dve", lambda e: e.reduce_max(out=s_[:, 0:1], in_=row_ap, axis=AX.X), r=[row_res], w=[s_])
        if clamp:
            kb.ts("dve", s_[:, 1:2], s_[:, 0:1], -1.0e20, ALU.max, r=[s_], w=[s_], s2=-1.0, op1=ALU.mult)
        else:
            kb.ts("dve", s_[:, 1:2], s_[:, 0:1], -1.0, ALU.mult, r=[s_], w=[s_])
        kb.act(out_ap, row_ap, AF.Exp, r=[row_res, s_], w=[out_res, s_], bias=s_[:, 1:2], accum_out=s_[:, 2:3])
        kb.ts("dve", s_[:, 3:4], s_[:, 2:3], 1.0e-30, ALU.max, r=[s_], w=[s_])
        kb.op("dve", lambda e: e.reciprocal(out=s_[:, 3:4], in_=s_[:, 3:4]), r=[s_], w=[s_])

    for qi in range(NQ):
        t0 = qi * 128
        q = qrot.next()
        kb.dma(q[:], zF_C.ap()[0:512, t0:t0 + 128].rearrange("(j d) t -> d j t", d=128), r=[zF_C], w=[q])
        kb.ts("dve", q[:], q[:], 128 ** -0.5, ALU.mult, r=[q], w=[q])
        gt = grot.next()
        kb.dma(gt[:], zT_C.ap()[t0:t0 + 128, 256:262], r=[zT_C], w=[gt], q="pool")
        kb.act(gt[:], gt[:], AF.Sigmoid, r=[gt], w=[gt])
        a_ = acc.next()
        nv = min(NCMP, 8 * qi + 7)
        nct = (nv + 127) // 128
        cb0 = 511 - 8 * qi
        for j in range(4):
            ps = psr.next()
            kb.mm(ps[:, 0:nv], q[:, j, :], kcmpT[:, 0:nv], True, True, r=[q, kcmpT], w=[ps])
            s1 = scc.next()
            kb.tt("dve", s1[:, 0:nv], ps[:, 0:nv], CB[:, j, cb0:cb0 + nv], ALU.add, r=[ps, CB], w=[s1])
            s_ = st.next()
            p = prot.next()
            softmax_stats(s_, s1[:, 0:nv], s1, p[:, 0:nv], p, True)
            kb.ts("dve", p[:, 0:nv], p[:, 0:nv], s_[:, 3:4], ALU.mult, r=[p, s_], w=[p])
            pst = psr.next()
            for nt in range(nct):
                nn = min(128, nv - nt * 128)
                kb.transpose(pst[:nn, nt * 128:(nt + 1) * 128], p[:, nt * 128:nt * 128 + nn], ident[:], r=[p, ident], w=[pst])
            for nt in range(nct):
                nn = min(128, nv - nt * 128)
                evac(pT[:nn, j, nt, :], pst[:nn, nt * 128:(nt + 1) * 128], [pst], [pT])
        ps_imp = psr.next()
        k = 0
        for j in range(4):
            for nt in range(nct):
                nn = min(128, nv - nt * 128)
                kb.mm(ps_imp[:, 0:NSEL], pT[:nn, j, nt, :], OV[:nn, nt, 0:NSEL], k == 0, k == 4 * nct - 1, r=[pT, OV], w=[ps_imp])
                k += 1
        for j in range(2):
            ps_o = psr.next()
            for nt in range(nct):
                nn = min(128, nv - nt * 128)
                kb.mm(ps_o[:, 0:128], pT[:nn, j, nt, :], vcmp[:nn, nt, :], nt == 0, nt == nct - 1, r=[pT, vcmp], w=[ps_o])
            kb.ts("dve", a_[:, j * 128:(j + 1) * 128], ps_o[:, 0:128], gt[:, j:j + 1], ALU.mult, r=[ps_o, gt], w=[a_])
        c0 = 127 - 2 * qi
        kb.tt("dve", sco[:, 0:NSEL], ps_imp[:, 0:NSEL], CM[:, c0:c0 + NSEL], ALU.add, r=[ps_imp, CM], w=[sco])
        kb.ts("dve", sco[:, 0:1], sco[:, 0:1], 1.0e9, ALU.add, r=[sco], w=[sco])
        kb.op("dve", lambda e: e.max(out=m8a[:], in_=sco[:, 0:NSEL]), r=[sco], w=[m8a])
        kb.op("dve", lambda e: e.match_replace(out=sco2[:, 0:NSEL], in_to_replace=m8a[:], in_values=sco[:, 0:NSEL], imm_value=-3.0e38), r=[sco, m8a], w=[sco2])
        kb.op("dve", lambda e: e.max(out=m8b[:], in_=sco2[:, 0:NSEL]), r=[sco2], w=[m8b])
        kb.ts("dve", nm[:, 0:NSEL], sco[:, 0:NSEL], m8b[:, 7:8], ALU.is_lt, r=[sco, m8b], w=[nm], s2=NEG, op1=ALU.mult)
        for j in range(2):
            for s0 in range(0, t0, 512):
                N = min(512, t0 - s0)
                ps = psr.next()
                kb.mm(ps[:, 0:N], q[:, j, :], ksT[:, s0:s0 + N], True, True, r=[q, ksT], w=[ps])
                i = (t0 - s0) // 128
                kb.stt(sc[:, s0:s0 + N], ps[:, 0:N], SL[:, j, i:i + 1], RS[:, j, 0:N], ALU.add, ALU.add, r=[ps, SL, RS], w=[sc])
            ps = psr.next()
            kb.mm(ps[:, 0:128], q[:, j, :], ksT[:, t0:t0 + 128], True, True, r=[q, ksT], w=[ps])
            kb.tt("dve", sc[:, t0:t0 + 128], ps[:, 0:128], RD[:, j, :], ALU.add, r=[ps, RD], w=[sc])
            NK = t0 + 128
            scv = sc[:, 0:NK].rearrange("p (m s) -> p m s", s=64)
            kb.tt("pool", scv, scv, nm[:, 0:NK // 64].unsqueeze(2).broadcast_to([128, NK // 64, 64]), ALU.add, r=[sc, nm], w=[sc])
            s_ = st.next()
            softmax_stats(s_, sc[:, 0:NK], sc, sc[:, 0:NK], sc, False)
            ps_o = psr.next()
            nkt = qi + 1
            for k4 in range(0, nkt, 4):
                kk = min(4, nkt - k4)
                pst = psr.next()
                for u in range(kk):
                    kt = k4 + u
                    kb.transpose(pst[:, u * 128:(u + 1) * 128], sc[:, kt * 128:(kt + 1) * 128], ident[:], r=[sc, ident], w=[pst])
                e = eT.next()
                evac(e[:, 0:kk * 128], pst[:, 0:kk * 128], [pst], [e])
                for u in range(kk):
                    kt = k4 + u
                    kb.mm(ps_o[:, 0:128], e[:, u * 128:(u + 1) * 128], vs[:, kt, :], kt == 0, kt == nkt - 1, r=[e, vs], w=[ps_o])
            kb.tt("dve", s_[:, 4:5], s_[:, 3:4], gt[:, 2 + j:3 + j], ALU.mult, r=[s_, gt], w=[s_])
            kb.stt(a_[:, j * 128:(j + 1) * 128], ps_o[:, 0:128], s_[:, 4:5], a_[:, j * 128:(j + 1) * 128], ALU.mult, ALU.add, r=[ps_o, s_, a_], w=[a_])
        ks = max(0, t0 - 512)
        NK = t0 + 128 - ks
        u0 = ks - (t0 - 512)
        kw = kwrot.next(); vw = vwrot.next()
        kb.dma(kw[:, 0:NK], zF_C.ap()[896:1024, ks:t0 + 128], r=[zF_C], w=[kw])
        kb.dma(vw[:, 0:NK // 128, :], zT_C.ap()[ks:t0 + 128, 128:256].rearrange("(c p) d -> p c d", p=128), r=[zT_C], w=[vw], q="pool")
        for j in range(2):
            sw_ = scw.next()
            for n0 in range(0, NK, 512):
                N = min(512, NK - n0)
                ps = psr.next()
                kb.mm(ps[:, 0:N], q[:, j, :], kw[:, n0:n0 + N], True, True, r=[q, kw], w=[ps])
                kb.tt("dve", sw_[:, n0:n0 + N], ps[:, 0:N], WB[:, j, u0 + n0:u0 + n0 + N], ALU.add, r=[ps, WB], w=[sw_])
            s_ = st.next()
            softmax_stats(s_, sw_[:, 0:NK], sw_, sw_[:, 0:NK], sw_, False)
            ps_o = psr.next()
            nkt = NK // 128
            for k4 in range(0, nkt, 4):
                kk = min(4, nkt - k4)
                pst = psr.next()
                for u in range(kk):
                    kt = k4 + u
                    kb.transpose(pst[:, u * 128:(u + 1) * 128], sw_[:, kt * 128:(kt + 1) * 128], ident[:], r=[sw_, ident], w=[pst])
                e = eT.next()
                evac(e[:, 0:kk * 128], pst[:, 0:kk * 128], [pst], [e])
                for u in range(kk):
                    kt = k4 + u
                    kb.mm(ps_o[:, 0:128], e[:, u * 128:(u + 1) * 128], vw[:, kt, :], kt == 0, kt == nkt - 1, r=[e, vw], w=[ps_o])
            kb.tt("dve", s_[:, 4:5], s_[:, 3:4], gt[:, 4 + j:5 + j], ALU.mult, r=[s_, gt], w=[s_])
            kb.stt(a_[:, j * 128:(j + 1) * 128], ps_o[:, 0:128], s_[:, 4:5], a_[:, j * 128:(j + 1) * 128], ALU.mult, ALU.add, r=[ps_o, s_, a_], w=[a_])
        kb.dma(y.ap()[t0:t0 + 128, ycol0:ycol0 + 256], a_[:], r=[a_], w=[y])
    kb.pop()
    kb.pop()


def nsa_tables(slopes4):
    p = np.arange(128, dtype=np.float64)[:, None]
    tabs = {}
    mi = np.arange(520, dtype=np.float64)[None, :]
    m = mi - 511
    dist = p - 16 * m - 31
    CB = np.zeros((128, 4, 520), np.float32)
    for j in range(4):
        CB[:, j, :] = np.where(dist >= 0, -slopes4[j] * dist, NEG)
    tabs["CB"] = CB
    c = np.arange(512, dtype=np.float64)[None, :]
    RS = np.zeros((128, 2, 512), np.float32); RD = np.zeros((128, 2, 128), np.float32)
    WB = np.zeros((128, 2, 640), np.float32); SL = np.zeros((128, 2, 64), np.float32)
    u = np.arange(640, dtype=np.float64)[None, :]
    dw = p + 512 - u
    for j in range(2):
        RS[:, j, :] = -slopes4[j] * (p - c)
        RD[:, j, :] = np.where(c[:, :128] <= p, -slopes4[j] * (p - c[:, :128]), NEG)
        WB[:, j, :] = np.where((dw >= 0) & (dw < 512), -slopes4[j] * dw, NEG)
        SL[:, j, :] = -slopes4[j] * 128.0 * np.arange(64)[None, :]
    tabs["RS"] = RS; tabs["RD"] = RD; tabs["WB"] = WB; tabs["SL"] = SL
    uu = np.arange(255)[None, :]
    rel = uu - 127
    cur = (np.arange(128)[:, None] >= 64).astype(np.int64)
    CM = np.where((rel == cur) | (rel == cur - 1), 1.0e9, np.where(rel > cur, NEG, 0.0)).astype(np.float32)
    tabs["CM"] = CM
    n = np.arange(512)[:, None]; ms = np.arange(128)[None, :]
    ov = ((16 * n < 64 * ms + 64) & (16 * n + 32 > 64 * ms)).astype(np.float32)
    tabs["OV"] = np.ascontiguousarray(ov.reshape(4, 128, 128).transpose(1, 0, 2))
    return tabs

EPS = 1e-6
GC = 0.7978845608028654


def gelu_tanh(kb, out_ap, x_ap, x_res, out_res, t1, W):
    kb.act(t1[:, :W], x_ap, AF.Square, r=[x_res], w=[t1])
    kb.ts("dve", t1[:, :W], t1[:, :W], 0.044715, ALU.mult, r=[t1], w=[t1], s2=1.0, op1=ALU.add)
    kb.tt("dve", t1[:, :W], t1[:, :W], x_ap, ALU.mult, r=[t1, x_res], w=[t1])
    kb.act(t1[:, :W], t1[:, :W], AF.Sigmoid, r=[t1], w=[t1], scale=2.0 * GC)
    kb.tt("dve", out_ap, t1[:, :W], x_ap, ALU.mult, r=[t1, x_res], w=[out_res])


def mixer_sgu(kb, zT_D, sgn, swT, sb_, cst, y, ycol0, T):
    NCH = T // 128
    kb.push()
    gn = kb.sb("gn", [128, 256]); kb.dma(gn[:], sgn.ap(), r=[sgn], w=[gn])
    wT = kb.sb("wT", [128, 2, 128]); kb.dma(wT[:], swT.ap(), r=[swT], w=[wT])
    tri = kb.sb("tri", [128, 128]); kb.dma(tri[:], cst["triT"].ap(), r=[cst["triT"]], w=[tri])
    bb = kb.sb("bb", [128, 2]); kb.dma(bb[:], sb_.ap(), r=[sb_], w=[bb])
    for g in range(2):
        kb.tt("dve", wT[:, g, :], wT[:, g, :], tri[:], ALU.mult, r=[wT, tri], w=[wT])
    zrot = Rot(kb, "z", [128, 1280], F32, 2)
    grot = Rot(kb, "g", [128, 1280], F32, 2)
    t1 = kb.sb("t1", [128, 1280])
    junk = kb.sb("junk", [128, 1024])
    vn = Rot(kb, "vn", [128, 256], F32, 2)
    yrot = Rot(kb, "y", [128, 256], F32, 2)
    ssq = kb.sb("ssq", [128, 2]); rs = kb.sb("rs", [128, 1])
    psr = Rot(kb, "ps", [128, 512], F32, 4, kind="ps")
    for c in range(NCH):
        t0 = c * 128
        z = zrot.next()
        kb.dma(z[:, 0:640], zT_D.ap()[t0:t0 + 128, 0:640], r=[zT_D], w=[z])
        kb.dma(z[:, 640:1280], zT_D.ap()[t0:t0 + 128, 640:1280], r=[zT_D], w=[z], q="pool")
        gl = grot.next()
        gelu_tanh(kb, gl[:], z[:], z, gl, t1, 1280)
        kb.act(junk[:, 0:1024], gl[:, 256:1280], AF.Square, r=[gl], w=[junk, ssq], accum_out=ssq[:, 0:1])
        kb.ts("dve", rs[:], ssq[:, 0:1], 1.0 / 1024, ALU.mult, r=[ssq], w=[rs], s2=EPS, op1=ALU.add)
        kb.act(rs[:], rs[:], AF.Sqrt, r=[rs], w=[rs])
        kb.op("dve", lambda e: e.reciprocal(out=rs[:], in_=rs[:]), r=[rs], w=[rs])
        v = vn.next()
        kb.stt(v[:], gl[:, 256:512], rs[:, 0:1], gn[:], ALU.mult, ALU.mult, r=[gl, rs, gn], w=[v])
        yt = yrot.next()
        for g in range(2):
            ps = psr.next()
            kb.mm(ps[:, 0:128], wT[:, g, :], v[:, g * 128:(g + 1) * 128], True, True, r=[wT, v], w=[ps])
            kb.stt(yt[:, g * 128:(g + 1) * 128], ps[:, 0:128], bb[:, g:g + 1], gl[:, g * 128:(g + 1) * 128], ALU.add, ALU.mult, r=[ps, bb, gl], w=[yt])
        kb.dma(y.ap()[t0:t0 + 128, ycol0:ycol0 + 256], yt[:], r=[yt], w=[y])
    kb.pop()


RMS_EPS = 1e-6
NEXP = 64
FF = 256


def phase_norm2(kb, xT, gcol, hT, D, T, out_dt=BF16, router=None, TT_=512):
    KC = D // 128
    kb.push()
    g_sb = kb.sb("g", [128, KC])
    kb.dma(g_sb[:], gcol.ap(), r=[gcol], w=[g_sb])
    ones = kb.sb("ones", [128, 128])
    kb.op("dve", lambda e: e.memset(ones[:], 1.0), w=[ones])
    xt = kb.sb("xt", [128, KC, TT_])
    sq = Rot(kb, "sq", [128, TT_], F32, 3)
    ssp = Rot(kb, "ssp", [128, TT_], F32, 2, kind="ps")
    rstd = kb.sb("rstd", [128, TT_])
    hb = Rot(kb, "hb", [128, KC, TT_], out_dt, 2 if out_dt == BF16 else 1)
    if router is not None:
        Wr, logits = router
        wr = kb.sb("wr", [128, KC, 72])
        kb.dma(wr[:], Wr.ap(), r=[Wr], w=[wr])
        hf = Rot(kb, "hf", [128, TT_], F32, 3)
        psr = [kb.ps("psr%d" % i, [128, 512], F32) for i in range(TT_ // 128)]
        lg = Rot(kb, "lg", [128, 72], F32, 2)
    xv = xT.ap().rearrange("(c p) t -> p c t", p=128)
    hv = hT.ap().rearrange("(c p) t -> p c t", p=128)
    NP = max(1, KC // 8)
    for ti in range(T // TT_):
        t0 = ti * TT_
        for j in range(NP):
            c0, c1 = j * KC // NP, (j + 1) * KC // NP
            kb.dma(xt[:, c0:c1, :], xv[:, c0:c1, t0:t0 + TT_], r=[xT], w=[xt], q=("sp" if j % 2 == 0 else "pool"))
        ss = ssp.next()
        for kc in range(KC):
            s = sq.next()
            kb.act(s[:], xt[:, kc, :], AF.Square, r=[xt], w=[s])
            kb.mm(ss[:], ones[:], s[:], kc == 0, kc == KC - 1, r=[ones, s], w=[ss])
        kb.ts("dve", rstd[:], ss[:], 1.0 / D, ALU.mult, r=[ss], w=[rstd], s2=RMS_EPS, op1=ALU.add)
        kb.act(rstd[:], rstd[:], AF.Sqrt, r=[rstd], w=[rstd])
        kb.op("dve", lambda e: e.reciprocal(out=rstd[:], in_=rstd[:]), r=[rstd], w=[rstd])
        h = hb.next()
        for kc in range(KC):
            if router is None:
                kb.stt(h[:, kc, :], xt[:, kc, :], g_sb[:, kc:kc + 1], rstd[:], ALU.mult, ALU.mult, r=[xt, g_sb, rstd], w=[h])
            else:
                f = hf.next()
                kb.stt(f[:], xt[:, kc, :], g_sb[:, kc:kc + 1], rstd[:], ALU.mult, ALU.mult, r=[xt, g_sb, rstd], w=[f])
                kb.copy("act", h[:, kc, :], f[:], r=[f], w=[h])
                for sub in range(TT_ // 128):
                    kb.mm(psr[sub][:, 0:72], f[:, sub * 128:(sub + 1) * 128], wr[:, kc, :], kc == 0, kc == KC - 1, r=[f, wr], w=[psr[sub]])
        if router is not None:
            for sub in range(TT_ // 128):
                l_ = lg.next()
                kb.copy("dve", l_[:], psr[sub][:, 0:72], r=[psr[sub]], w=[l_])
                kb.dma(logits.ap()[t0 + sub * 128:t0 + (sub + 1) * 128, :], l_[:], r=[l_], w=[logits])
        for j in range(NP):
            c0, c1 = j * KC // NP, (j + 1) * KC // NP
            kb.dma(hv[:, c0:c1, t0:t0 + TT_], h[:, c0:c1, :], r=[h], w=[hT], q=("sp" if j % 2 == 0 else "pool"))
    kb.pop()


def phase_merge(kb, hT, yT, Wg, Wb, mT, D, T):
    KC = D // 128; KB_ = KC // 4; NDT = D // 128
    NT = min(1024, T)
    kb.push()
    hs = kb.sb("hs", [128, KC, NT], BF16)
    ys = kb.sb("ys", [128, KC, NT], BF16)
    wg = Rot(kb, "wg", [128, KC, 128], BF16, 3)
    wb = Rot(kb, "wb", [128, KB_, 128], BF16, 3)
    macc = Rot(kb, "macc", [128, NT], F32, 2)
    sg = Rot(kb, "sg", [128, 512], F32, 3)
    tmp = Rot(kb, "tmp", [128, 512], F32, 3)
    mo = Rot(kb, "mo", [128, NT], BF16, 2)
    psr = Rot(kb, "ps", [128, 512], F32, 6, kind="ps")
    hv = hT.ap().rearrange("(c p) t -> p c t", p=128)
    yv = yT.ap().rearrange("(c p) t -> p c t", p=128)
    mv = mT.ap().rearrange("(c p) t -> p c t", p=128)
    for h0 in range(0, T, NT):
        for j in range(0, KC, 8):
            kb.dma(hs[:, j:min(j + 8, KC), :], hv[:, j:min(j + 8, KC), h0:h0 + NT], r=[hT], w=[hs])
            kb.dma(ys[:, j:min(j + 8, KC), :], yv[:, j:min(j + 8, KC), h0:h0 + NT], r=[yT], w=[ys], q="pool")
        for dt in range(NDT):
            m = macc.next()
            for n in range(4):
                g_ = wg.next(); b_ = wb.next()
                kb.dma(g_[:], Wg.ap()[n, dt], r=[Wg], w=[g_], q="pool")
                kb.dma(b_[:], Wb.ap()[n, dt], r=[Wb], w=[b_], q="pool")
                for tt in range(NT // 512):
                    ts_ = slice(tt * 512, (tt + 1) * 512)
                    pg = psr.next()
                    for kc in range(KC):
                        kb.mm(pg[:], g_[:, kc, :], hs[:, kc, ts_], kc == 0, kc == KC - 1, r=[g_, hs], w=[pg])
                    pb = psr.next()
                    for kc in range(KB_):
                        kb.mm(pb[:], b_[:, kc, :], ys[:, n * KB_ + kc, ts_], kc == 0, kc == KB_ - 1, r=[b_, ys], w=[pb])
                    s = sg.next()
                    kb.act(s[:], pg[:], AF.Sigmoid, r=[pg], w=[s])
                    if n == 0:
                        kb.tt("dve", m[:, ts_], s[:], pb[:], ALU.mult, r=[s, pb], w=[m])
                    else:
                        t_ = tmp.next()
                        kb.tt("dve", t_[:], s[:], pb[:], ALU.mult, r=[s, pb], w=[t_])
                        kb.tt("pool", m[:, ts_], m[:, ts_], t_[:], ALU.add, r=[m, t_], w=[m])
            o = mo.next()
            kb.copy("act", o[:], m[:], r=[m], w=[o])
            kb.dma(mv[:, dt, h0:h0 + NT], o[:], r=[o], w=[mT])
    kb.pop()


def phase_wout(kb, mT, Wo, xT, x1T, D, T):
    KC = D // 128; NDT = D // 128
    NT = min(1024, T)
    kb.push()
    ms = kb.sb("ms", [128, KC, NT], BF16)
    wo = Rot(kb, "wo", [128, KC, 128], BF16, 3)
    xr = Rot(kb, "xr", [128, NT], F32, 3)
    psr = Rot(kb, "ps", [128, 512], F32, 4, kind="ps")
    mv = mT.ap().rearrange("(c p) t -> p c t", p=128)
    xv = xT.ap().rearrange("(c p) t -> p c t", p=128)
    ov = x1T.ap().rearrange("(c p) t -> p c t", p=128)
    for h0 in range(0, T, NT):
        for j in range(0, KC, 8):
            kb.dma(ms[:, j:min(j + 8, KC), :], mv[:, j:min(j + 8, KC), h0:h0 + NT], r=[mT], w=[ms])
        for dt in range(NDT):
            w_ = wo.next()
            kb.dma(w_[:], Wo.ap()[dt], r=[Wo], w=[w_], q="pool")
            x_ = xr.next()
            kb.dma(x_[:], xv[:, dt, h0:h0 + NT], r=[xT], w=[x_])
            for tt in range(NT // 512):
                ts_ = slice(tt * 512, (tt + 1) * 512)
                p = psr.next()
                for kc in range(KC):
                    kb.mm(p[:], w_[:, kc, :], ms[:, kc, ts_], kc == 0, kc == KC - 1, r=[w_, ms], w=[p])
                kb.tt("dve", x_[:, ts_], x_[:, ts_], p[:], ALU.add, r=[x_, p], w=[x_])
            kb.dma(ov[:, dt, h0:h0 + NT], x_[:], r=[x_], w=[x1T])
    kb.pop()


def phase_route(kb, logits, rbias, combT, cst, T):
    kb.push()
    ident = kb.sb("ident", [128, 128]); kb.dma(ident[:], cst["ident"].ap(), r=[cst["ident"]], w=[ident])
    rb = kb.sb("rb", [128, 72]); kb.dma(rb[:], rbias.ap(), r=[rbias], w=[rb])
    cT = kb.sb("cT", [64, T])
    lgr = Rot(kb, "lg", [128, 72], F32, 2)
    sm = Rot(kb, "sm", [128, 16], F32, 2)
    oh = Rot(kb, "oh", [128, 8], F32, 2)
    ge = Rot(kb, "ge", [128, 8], F32, 2)
    mk = Rot(kb, "mk", [128, 64], F32, 2)
    m8 = Rot(kb, "m8", [128, 8], F32, 2)
    cb = Rot(kb, "cb", [128, 64], F32, 2)
    cb2 = Rot(kb, "cb2", [128, 64], F32, 2)
    psr = Rot(kb, "ps", [128, 512], F32, 2, kind="ps")
    for ti in range(T // 128):
        t0 = ti * 128
        lg = lgr.next()
        kb.dma(lg[:], logits.ap()[t0:t0 + 128, :], r=[logits], w=[lg])
        kb.tt("dve", lg[:], lg[:], rb[:], ALU.add, r=[lg, rb], w=[lg])
        s = sm.next()
        kb.op("dve", lambda e: e.reduce_max(out=s[:, 0:1], in_=lg[:, 0:8], axis=AX.X), r=[lg], w=[s])
        o = oh.next()
        kb.ts("dve", o[:], lg[:, 0:8], s[:, 0:1], ALU.is_equal, r=[lg, s], w=[o])
        kb.ts("dve", o[:], o[:], 1.0e30, ALU.mult, r=[o], w=[o], s2=-1.0e30, op1=ALU.add)
        kb.ts("dve", s[:, 1:2], s[:, 0:1], -1.0, ALU.mult, r=[s], w=[s])
        g = ge.next()
        kb.act(g[:], lg[:, 0:8], AF.Exp, r=[lg, s], w=[g, s], bias=s[:, 1:2], accum_out=s[:, 2:3])
        kb.op("dve", lambda e: e.reciprocal(out=s[:, 3:4], in_=s[:, 2:3]), r=[s], w=[s])
        m = mk.next()
        kb.tt("dve", m[:].rearrange("p (g e) -> p g e", e=8), lg[:, 8:72].rearrange("p (g e) -> p g e", e=8),
              o[:].unsqueeze(2).broadcast_to([128, 8, 8]), ALU.add, r=[lg, o], w=[m])
        t8 = m8.next()
        kb.op("dve", lambda e: e.max(out=t8[:], in_=m[:]), r=[m], w=[t8])
        kb.tt("dve", s[:, 4:5], t8[:, 1:2], t8[:, 0:1], ALU.subtract, r=[t8], w=[s])
        kb.act(s[:, 5:6], s[:, 4:5], AF.Exp, r=[s], w=[s])
        kb.ts("dve", s[:, 6:7], s[:, 5:6], 1.0, ALU.add, r=[s], w=[s])
        kb.op("dve", lambda e: e.reciprocal(out=s[:, 6:7], in_=s[:, 6:7]), r=[s], w=[s])
        kb.tt("dve", s[:, 7:8], s[:, 5:6], s[:, 6:7], ALU.mult, r=[s], w=[s])
        kb.tt("dve", s[:, 8:9], s[:, 6:7], s[:, 3:4], ALU.mult, r=[s], w=[s])
        kb.tt("dve", s[:, 9:10], s[:, 7:8], s[:, 3:4], ALU.mult, r=[s], w=[s])
        c = cb.next(); c2 = cb2.next()
        kb.ts("dve", c[:], m[:], t8[:, 0:1], ALU.is_equal, r=[m, t8, s], w=[c], s2=s[:, 8:9], op1=ALU.mult)
        kb.ts("dve", c2[:], m[:], t8[:, 1:2], ALU.is_equal, r=[m, t8, s], w=[c2], s2=s[:, 9:10], op1=ALU.mult)
        kb.tt("dve", c[:], c[:], c2[:], ALU.add, r=[c, c2], w=[c])
        p = psr.next()
        kb.transpose(p[0:64, 0:128], c[:], ident[:], r=[c, ident], w=[p])
        kb.copy("act", cT[:, t0:t0 + 128], p[0:64, 0:128], r=[p], w=[cT])
    kb.dma(combT.ap(), cT[:], r=[cT], w=[combT])
    kb.pop()


def phase_moe(kb, h2T, x1T, combT, W1, W3, W2, x2T, D, T):
    KC = D // 128; NDT = D // 128
    TT_ = 512
    kb.push()
    acc = kb.sb("acc", [128, KC, TT_])
    accr = [Res("acc%d" % i) for i in range(KC)]
    hs = kb.sb("hs", [128, KC, TT_], BF16)
    w1r = Rot(kb, "w1", [128, KC, FF], BF16, 2)
    w3r = Rot(kb, "w3", [128, KC, FF], BF16, 2)
    w2r = Rot(kb, "w2", [128, 2, D], BF16, 2)
    cbr = Rot(kb, "cb", [128, TT_], F32, 2)
    sr = Rot(kb, "s", [128, TT_], F32, 3)
    atr = Rot(kb, "at", [128, 2, TT_], BF16, 2)
    psr = Rot(kb, "ps", [128, 512], F32, 7, kind="ps")
    hv = h2T.ap().rearrange("(c p) t -> p c t", p=128)
    xv = x1T.ap().rearrange("(c p) t -> p c t", p=128)
    ov = x2T.ap().rearrange("(c p) t -> p c t", p=128)
    for ti in range(T // TT_):
        t0 = ti * TT_
        for j in range(0, KC, 8):
            kb.dma(hs[:, j:min(j + 8, KC), :], hv[:, j:min(j + 8, KC), t0:t0 + TT_], r=[h2T], w=[hs])
            kb.dma(acc[:, j:min(j + 8, KC), :], xv[:, j:min(j + 8, KC), t0:t0 + TT_], r=[x1T], w=accr[j:min(j + 8, KC)])
        for e in range(NEXP):
            w1 = w1r.next(); w3 = w3r.next(); w2 = w2r.next()
            kb.dma(w1[:], W1.ap()[e].rearrange("(c p) f -> p c f", p=128), r=[W1], w=[w1], q="pool")
            kb.dma(w3[:], W3.ap()[e].rearrange("(c p) f -> p c f", p=128), r=[W3], w=[w3], q="pool")
            kb.dma(w2[:], W2.ap()[e].rearrange("(f p) d -> p f d", p=128), r=[W2], w=[w2], q="pool")
            cb = cbr.next()
            kb.dma(cb[:], combT.ap()[e:e + 1, t0:t0 + TT_].broadcast_to([128, TT_]), r=[combT], w=[cb])
            at = atr.next()
            for ft in range(2):
                p1 = psr.next()
                for kc in range(KC):
                    kb.mm(p1[:], w1[:, kc, ft * 128:(ft + 1) * 128], hs[:, kc, :], kc == 0, kc == KC - 1, r=[w1, hs], w=[p1])
                p3 = psr.next()
                for kc in range(KC):
                    kb.mm(p3[:], w3[:, kc, ft * 128:(ft + 1) * 128], hs[:, kc, :], kc == 0, kc == KC - 1, r=[w3, hs], w=[p3])
                s = sr.next()
                kb.act(s[:], p1[:], AF.Silu, r=[p1], w=[s])
                kb.tt("dve", s[:], s[:], p3[:], ALU.mult, r=[s, p3], w=[s])
                kb.tt("pool", at[:, ft, :], s[:], cb[:], ALU.mult, r=[s, cb], w=[at])
            for dt in range(NDT):
                p = psr.next()
                for ft in range(2):
                    kb.mm(p[:], w2[:, ft, dt * 128:(dt + 1) * 128], at[:, ft, :], ft == 0, ft == 1, r=[w2, at], w=[p])
                kb.tt("dve", acc[:, dt, :], acc[:, dt, :], p[:], ALU.add, r=[accr[dt], p], w=[accr[dt]])
        for j in range(0, KC, 8):
            kb.dma(ov[:, j:min(j + 8, KC), t0:t0 + TT_], acc[:, j:min(j + 8, KC), :], r=accr[j:min(j + 8, KC)], w=[x2T], q=("sp" if (j // 8) % 2 == 0 else "act"))
    kb.pop()

D_MODEL = 4096
SEQ = 8192
BATCH = 2
MW = 1024
OFF = {}
_o = 0
for _n, _w in (("aq", 1024), ("ak", 1024), ("av", 1024), ("ao", 1024), ("ai", 8), ("af", 8),
               ("bq", 1024), ("bk", 1024), ("bv", 1024), ("bg", 1024),
               ("cq", 1024), ("ckc", 256), ("cvc", 256), ("cks", 256), ("cvs", 256), ("ckw", 256), ("cvw", 256), ("cg", 24),
               ("du", 1024), ("dv", 1024), ("merge", 16384)):
    OFF[_n] = _o
    _o += _w
NCOLS1 = 4618
GROUPS1 = [
    (0, 512, "F", "zF_A", 0), (512, 4, "F", "zF_Ag", 0), (516, 512, "F", "zF_B", 0), (1028, 1024, "F", "zF_C", 0),
    (2052, 512, "T", "zT_A", 0), (2564, 512, "T", "zT_B", 0), (3076, 262, "T", "zT_C", 0), (3338, 1280, "T", "zT_D", 0)]

P1_INPUTS = {"xT": [D_MODEL, SEQ], "gmix": [128, 32], "W": [D_MODEL, NCOLS1], "convw": [128, 4, 4], "gbias": [64, 4],
             "gnA": [128, 256], "gnB": [128, 256], "rdec": [128, 2, 128], "rcols": [128, 2, 3],
             "cmpw": [128, 2, 32, 128], "cmppe": [128, 2, 32], "sgn": [128, 256], "swT": [128, 2, 128], "sgb": [128, 2],
             "ident": [128, 128], "bigmaskT": [128, 128], "triT": [128, 128],
             "tab_CB": [128, 4, 520], "tab_RS": [128, 2, 512], "tab_RD": [128, 2, 128], "tab_WB": [128, 2, 640],
             "tab_SL": [128, 2, 64], "tab_CM": [128, 255], "tab_OV": [128, 4, 128]}


def build_p1(T=SEQ, D=D_MODEL):
    nc = bass.Bass("TRN2", target_bir_lowering=False)
    kb = KB(nc)
    d = {}
    for k, shp in P1_INPUTS.items():
        shp = list(shp)
        if k == "xT":
            shp = [D, T]
        if k == "W":
            shp = [D, NCOLS1]
        if k == "gmix":
            shp = [128, D // 128]
        d[k] = kb.dram(k, shp, F32, kind="ExternalInput")
    y = kb.dram("y", [T, 1024], F32, kind="ExternalOutput")
    hT = kb.dram("hT", [D, T], BF16)
    z = {"zF_A": kb.dram("zF_A", [512, T]), "zF_Ag": kb.dram("zF_Ag", [4, T]), "zF_B": kb.dram("zF_B", [512, T]),
         "zF_C": kb.dram("zF_C", [1024, T]), "zT_A": kb.dram("zT_A", [T, 512]), "zT_B": kb.dram("zT_B", [T, 512]),
         "zT_C": kb.dram("zT_C", [T, 262]), "zT_D": kb.dram("zT_D", [T, 1280])}
    scr = kb.dram("scr", [2, T])
    phase_norm(kb, d["xT"], d["gmix"], hT, D, T)
    phase_proj(kb, hT, d["W"], [(c0, w, kind, z[dn], do) for (c0, w, kind, dn, do) in GROUPS1], D, T)
    cst = {"ident": d["ident"], "bigmaskT": d["bigmaskT"], "triT": d["triT"]}
    mixer_mlstm(kb, z["zF_A"], z["zF_Ag"], z["zT_A"], d["convw"], d["gbias"], d["gnA"], cst, y, 0, T, scr)
    mixer_ret(kb, z["zF_B"], z["zT_B"], d["gnB"], cst, d["rdec"], d["rcols"], y, 256, T)
    mixer_nsa(kb, z["zF_C"], z["zT_C"], d["cmpw"], d["cmppe"], {k[4:]: d[k] for k in d if k.startswith("tab_")}, cst, y, 512, T)
    mixer_sgu(kb, z["zT_D"], d["sgn"], d["swT"], d["sgb"], cst, y, 768, T)
    kb.barrier()
    kb.es.close()
    return nc


def build_p2(TK, final, D=D_MODEL):
    nc = bass.Bass("TRN2", target_bir_lowering=False)
    kb = KB(nc)
    KC = D // 128
    shapes = {"xT": [D, TK], "yT": [D, TK], "gmix": [128, KC], "gffn": [128, KC], "gfin": [128, KC],
              "Wg": [4, KC, 128, KC, 128], "Wb": [4, KC, 128, KC // 4, 128], "Wo": [KC, 128, KC, 128],
              "Wr": [128, KC, 72], "rbias": [128, 72], "W1": [64, D, 256], "W3": [64, D, 256], "W2": [64, 256, D],
              "ident": [128, 128]}
    d = {k: kb.dram(k, s, F32, kind="ExternalInput") for k, s in shapes.items()}
    if final:
        x2T = kb.dram("x2T", [D, TK], F32)
        outT = kb.dram("outT", [D, TK], F32, kind="ExternalOutput")
    else:
        x2T = kb.dram("x2T", [D, TK], F32, kind="ExternalOutput")
    x1T = kb.dram("x1T", [D, TK], F32)
    hT = kb.dram("hT", [D, TK], BF16); mT = kb.dram("mT", [D, TK], BF16)
    h2T = kb.dram("h2T", [D, TK], BF16); logits = kb.dram("logits", [TK, 72], F32); combT = kb.dram("combT", [64, TK], F32)
    phase_norm2(kb, d["xT"], d["gmix"], hT, D, TK)
    phase_merge(kb, hT, d["yT"], d["Wg"], d["Wb"], mT, D, TK)
    phase_wout(kb, mT, d["Wo"], d["xT"], x1T, D, TK)
    phase_norm2(kb, x1T, d["gffn"], h2T, D, TK, router=(d["Wr"], logits))
    phase_route(kb, logits, d["rbias"], combT, {"ident": d["ident"]}, TK)
    phase_moe(kb, h2T, x1T, combT, d["W1"], d["W3"], d["W2"], x2T, D, TK)
    if final:
        phase_norm2(kb, x2T, d["gfin"], outT, D, TK, out_dt=F32)
    kb.barrier()
    kb.es.close()
    return nc


def _bc(v, n=128):
    return np.ascontiguousarray(np.broadcast_to(np.asarray(v, np.float32), (n,) + np.asarray(v).shape))


def _gcol(g):
    return np.ascontiguousarray(np.asarray(g, np.float32).reshape(-1, 128).T)


def p1_core_inputs(l, hp, inp, const):
    h0, h1 = 2 * hp, 2 * hp + 1
    G = hp // 2
    others = [h for h in range(4 * G, 4 * G + 4) if h not in (h0, h1)]
    slots = [h0, h1] + others
    w_in = inp["w_in"][l]

    def hc(name, h):
        return list(range(OFF[name] + h * 128, OFF[name] + (h + 1) * 128))
    cols = []
    cols += hc("aq", h0) + hc("aq", h1) + hc("ak", h0) + hc("ak", h1)
    cols += [OFF["ai"] + h0, OFF["ai"] + h1, OFF["af"] + h0, OFF["af"] + h1]
    cols += hc("bq", h0) + hc("bq", h1) + hc("bk", h0) + hc("bk", h1)
    for s in slots:
        cols += hc("cq", s)
    cols += hc("ckc", G) + hc("cvc", G) + hc("cks", G) + hc("ckw", G)
    cols += hc("av", h0) + hc("av", h1) + hc("ao", h0) + hc("ao", h1)
    cols += hc("bv", h0) + hc("bv", h1) + hc("bg", h0) + hc("bg", h1)
    cols += hc("cvs", G) + hc("cvw", G)
    cols += [OFF["cg"] + t * 8 + h for t in range(3) for h in (h0, h1)]
    own = list(range(hp * 256, (hp + 1) * 256))
    oth = [c for c in range(1024) if c < hp * 256 or c >= (hp + 1) * 256]
    cols += [OFF["du"] + c for c in own] + [OFF["dv"] + c for c in own] + [OFF["dv"] + c for c in oth]
    assert len(cols) == NCOLS1
    m = {}
    m["W"] = np.ascontiguousarray(w_in[:, cols])
    m["gmix"] = _gcol(inp["norm_mix"][l])
    cw = inp["mlstm_conv"][l]
    cwq, cwk = cw[:, :1024], cw[:, 1024:]
    m["convw"] = np.ascontiguousarray(np.stack([cwq[:, h0 * 128:(h0 + 1) * 128].T, cwq[:, h1 * 128:(h1 + 1) * 128].T,
                                                cwk[:, h0 * 128:(h0 + 1) * 128].T, cwk[:, h1 * 128:(h1 + 1) * 128].T], 1))
    gb = inp["mlstm_gate_bias"][l]
    m["gbias"] = _bc(np.array([gb[0][h0], gb[0][h1], gb[1][h0], gb[1][h1]], np.float32), 64)
    m["gnA"] = _bc(inp["mlstm_norm"][l][h0 * 128:(h1 + 1) * 128])
    m["gnB"] = _bc(inp["ret_norm"][l][h0 * 128:(h1 + 1) * 128])
    m["cmpw"] = np.ascontiguousarray(inp["nsa_cmp_w"][l].transpose(2, 0, 1, 3))
    m["cmppe"] = np.ascontiguousarray(inp["nsa_cmp_pe"][l].transpose(2, 0, 1))
    m["sgn"] = _bc(inp["sgu_norm"][l][hp * 256:(hp + 1) * 256])
    sw = inp["sgu_w"][l]
    m["swT"] = np.ascontiguousarray(np.stack([sw[h0].T, sw[h1].T], 1))
    sbv = inp["sgu_b"][l]
    m["sgb"] = np.ascontiguousarray(np.stack([sbv[h0], sbv[h1]], 1))
    m.update(const[hp])
    return m


def host_consts():
    out = []
    s_ = np.arange(128)[:, None]; l_ = np.arange(128)[None, :]
    slopes = 2.0 ** (-8.0 * (np.arange(8) + 1.0) / 8)
    for hp in range(4):
        h0, h1 = 2 * hp, 2 * hp + 1
        G = hp // 2
        others = [h for h in range(4 * G, 4 * G + 4) if h not in (h0, h1)]
        c = {"ident": np.eye(128, dtype=np.float32), "bigmaskT": np.where(s_ > l_, 1e30, 0).astype(np.float32),
             "triT": (s_ <= l_).astype(np.float32)}
        rdec = np.zeros((128, 2, 128), np.float32); rcols = np.zeros((128, 2, 3), np.float32)
        for i, h in enumerate((h0, h1)):
            gam = 1.0 - 2.0 ** (-5.0 - h)
            rdec[:, i, :] = np.where(l_ >= s_, gam ** np.maximum(l_ - s_, 0), 0) * 128 ** -0.5
            rcols[:, i, 0] = gam ** (np.arange(128) + 1.0)
            rcols[:, i, 1] = gam ** (127.0 - np.arange(128)) * 128 ** -0.5
            rcols[:, i, 2] = gam ** 128
        c["rdec"] = rdec; c["rcols"] = rcols
        for k, v in nsa_tables([slopes[h] for h in [h0, h1] + others]).items():
            c["tab_" + k] = v
        out.append(c)
    return out


_PROGS = {}


def _prog(key, fn):
    if key not in _PROGS:
        _PROGS[key] = fn()
    return _PROGS[key]


def kernel(**inp):
    inp = {k: np.asarray(v) for k, v in inp.items()}
    x = inp["x"].astype(np.float32, copy=False)
    T = SEQ
    const = host_consts()
    ident = np.eye(128, dtype=np.float32)
    xT = [np.ascontiguousarray(x[b].T) for b in range(BATCH)]
    out = None
    for l in range(2):
        nc1 = _prog("p1", build_p1)
        per_hp = [p1_core_inputs(l, hp, inp, const) for hp in range(4)]
        in_maps = []
        for c in range(8):
            b, hp = c // 4, c % 4
            m = dict(per_hp[hp]); m["xT"] = xT[b]
            in_maps.append(m)
        res = run_bass_kernel_spmd(nc1, in_maps, core_ids=list(range(8)))
        ys = [r["y"] for r in res.results]
        del in_maps, per_hp
        final = (l == 1)
        nc2 = _prog("p2f" if final else "p2", lambda: build_p2(T, final))
        w_in = inp["w_in"][l]
        shared = {
            "gmix": _gcol(inp["norm_mix"][l]), "gffn": _gcol(inp["norm_ffn"][l]), "gfin": _gcol(inp["norm_final"]),
            "Wg": np.ascontiguousarray(w_in[:, OFF["merge"]:].reshape(32, 128, 4, 32, 128).transpose(2, 3, 1, 0, 4)),
            "Wb": np.ascontiguousarray(inp["w_branch"][l].reshape(4, 8, 128, 32, 128).transpose(0, 3, 2, 1, 4)),
            "Wo": np.ascontiguousarray(inp["w_out"][l].reshape(32, 128, 32, 128).transpose(2, 1, 0, 3)),
            "Wr": np.ascontiguousarray(np.concatenate([inp["router_group_w"][l], inp["router_expert_w"][l]], 1).reshape(32, 128, 72).transpose(1, 0, 2)),
            "rbias": _bc(np.concatenate([inp["router_group_b"][l], inp["router_expert_b"][l]])),
            "W1": np.ascontiguousarray(inp["expert_w1"][l]), "W3": np.ascontiguousarray(inp["expert_w3"][l]),
            "W2": np.ascontiguousarray(inp["expert_w2"][l]), "ident": ident}
        in_maps = []
        for b in range(BATCH):
            Y = np.stack([ys[b * 4 + hp].reshape(T, 4, 256) for hp in range(4)], 2)
            yT = np.ascontiguousarray(Y.reshape(T, 4096).T)
            m = dict(shared); m["xT"] = xT[b]; m["yT"] = yT
            in_maps.append(m)
        res = run_bass_kernel_spmd(nc2, in_maps, core_ids=list(range(BATCH)))
        del in_maps, shared, ys
        if final:
            out = np.stack([np.ascontiguousarray(res.results[b]["outT"].T) for b in range(BATCH)], 0)
        else:
            xT = [np.ascontiguousarray(res.results[b]["x2T"]) for b in range(BATCH)]
    return out.astype(np.float32, copy=False)
```
